# Optimizing a Trainium2 kernel written in Bass

```python
import jax, jax.numpy as jnp
from jax import lax
import numpy as np

D_MODEL = 2048
BATCH = 1
SEQ = 8192
DEPTH = 1

HEAD_DIM = 128
N_HEADS_MOBA = D_MODEL // (2 * HEAD_DIM)
N_HEADS_FOX = D_MODEL // (2 * HEAD_DIM)
MOBA_WIDTH = N_HEADS_MOBA * HEAD_DIM
FOX_WIDTH = N_HEADS_FOX * HEAD_DIM
MOBA_BLOCK = 256
MOBA_TOPK = 3
MOBA_Q_CHUNK = 64
FOX_Q_BLOCK = 128
D_FF = 4 * D_MODEL
RMS_EPS = 1e-6
IN_WIDTHS = (MOBA_WIDTH, MOBA_WIDTH, MOBA_WIDTH,
             FOX_WIDTH, FOX_WIDTH, FOX_WIDTH,
             N_HEADS_FOX,
             D_MODEL, D_MODEL)
D_IN = sum(IN_WIDTHS)

kernel_name = "moba_fox_gated_hybrid_layer"


def rms_norm(x, g):
    xf = x.astype(jnp.float32)
    y = xf * lax.rsqrt(jnp.mean(xf * xf, axis=-1, keepdims=True) + RMS_EPS)
    return (y * g.astype(jnp.float32)).astype(x.dtype)


def split_heads(t, n_heads):
    b, s, _ = t.shape
    return t.reshape(b, s, n_heads, HEAD_DIM).transpose(0, 2, 1, 3)


def merge_heads(t):
    b, h, s, d = t.shape
    return t.transpose(0, 2, 1, 3).reshape(b, s, h * d)


def to_chunks(a, c):
    a = a.reshape(a.shape[:2] + (a.shape[2] // c, c) + a.shape[3:])
    return jnp.moveaxis(a, 2, 0)


def from_chunks(a):
    a = jnp.moveaxis(a, 0, 2)
    return a.reshape(a.shape[:2] + (a.shape[2] * a.shape[3],) + a.shape[4:])


def alibi_slopes(n_heads):
    return jnp.exp2(-8.0 * jnp.arange(1, n_heads + 1, dtype=jnp.float32) / n_heads)


def moba_attention(q, k, v):
    b, h, s, d = q.shape
    s_pad = -(-s // MOBA_BLOCK) * MOBA_BLOCK
    pad = ((0, 0), (0, 0), (0, s_pad - s), (0, 0))
    q, k, v = jnp.pad(q, pad), jnp.pad(k, pad), jnp.pad(v, pad)
    nb = s_pad // MOBA_BLOCK
    n_sel = min(MOBA_TOPK, nb)
    scale = HEAD_DIM ** -0.5
    slopes = alibi_slopes(h)[None, :, None]
    kb = k.reshape(b, h, nb, MOBA_BLOCK, d)
    vb = v.reshape(b, h, nb, MOBA_BLOCK, d)
    kmean = jnp.mean(kb.astype(jnp.float32), axis=3)
    gate = jnp.einsum('bhtd,bhnd->bhtn', q.astype(jnp.float32), kmean)
    q_blk = jnp.arange(s_pad) // MOBA_BLOCK
    past = jnp.arange(nb)[None, :] < q_blk[:, None]
    gate = jnp.where(past, gate, -jnp.inf)
    top_vals, sel = lax.top_k(gate, n_sel)
    valid = jnp.isfinite(top_vals)
    bi = jnp.arange(b)[:, None, None, None]
    hi = jnp.arange(h)[None, :, None, None]
    blk_off = jnp.arange(MOBA_BLOCK)

    def chunk(args):
        ci, qc, selc, validc = args
        q_pos = ci * MOBA_Q_CHUNK + jnp.arange(MOBA_Q_CHUNK)
        own = (ci * MOBA_Q_CHUNK) // MOBA_BLOCK
        k_own = lax.dynamic_index_in_dim(kb, own, axis=2, keepdims=False)
        v_own = lax.dynamic_index_in_dim(vb, own, axis=2, keepdims=False)
        dist_own = (q_pos[:, None] - (own * MOBA_BLOCK + blk_off)[None, :])
        s_own = jnp.einsum('bhcd,bhsd->bhcs', qc, k_own).astype(jnp.float32) * scale
        s_own = s_own - slopes[..., None] * jnp.abs(dist_own).astype(jnp.float32)
        s_own = jnp.where(dist_own >= 0, s_own, -jnp.inf)
        k_sel = kb[bi, hi, selc]
        v_sel = vb[bi, hi, selc]
        s_sel = jnp.einsum('bhcd,bhcjsd->bhcjs', qc, k_sel).astype(jnp.float32) * scale
        sel_pos = selc[..., None] * MOBA_BLOCK + blk_off
        dist_sel = jnp.abs(q_pos[None, None, :, None, None] - sel_pos).astype(jnp.float32)
        s_sel = s_sel - slopes[..., None, None] * dist_sel
        s_sel = jnp.where(validc[..., None], s_sel, -jnp.inf)
        c, j = selc.shape[2], selc.shape[3]
        scores = jnp.concatenate([s_sel.reshape(b, h, c, j * MOBA_BLOCK), s_own], axis=-1)
        p = jax.nn.softmax(scores, axis=-1).astype(v.dtype)
        p_sel = p[..., :j * MOBA_BLOCK].reshape(b, h, c, j, MOBA_BLOCK)
        p_own = p[..., j * MOBA_BLOCK:]
        return (jnp.einsum('bhcjs,bhcjsd->bhcd', p_sel, v_sel)
                + jnp.einsum('bhcs,bhsd->bhcd', p_own, v_own))

    nc = s_pad // MOBA_Q_CHUNK
    out = lax.map(chunk, (jnp.arange(nc), to_chunks(q, MOBA_Q_CHUNK),
                          to_chunks(sel, MOBA_Q_CHUNK), to_chunks(valid, MOBA_Q_CHUNK)))
    return from_chunks(out)[:, :, :s]


def forgetting_attention(q, k, v, f_logit, b_forget):
    b, h, s, d = q.shape
    scale = HEAD_DIM ** -0.5
    log_f = jax.nn.log_sigmoid(f_logit.astype(jnp.float32) + b_forget.astype(jnp.float32))
    c = jnp.cumsum(log_f, axis=1).transpose(0, 2, 1)
    key_pos = jnp.arange(s)

    def chunk(args):
        ci, qc, cq = args
        sc = jnp.einsum('bhcd,bhsd->bhcs', qc, k).astype(jnp.float32) * scale
        sc = sc + cq[..., None] - c[:, :, None, :]
        q_pos = ci * FOX_Q_BLOCK + jnp.arange(FOX_Q_BLOCK)
        sc = jnp.where(q_pos[:, None] >= key_pos[None, :], sc, -jnp.inf)
        p = jax.nn.softmax(sc, axis=-1).astype(v.dtype)
        return jnp.einsum('bhcs,bhsd->bhcd', p, v)

    nc = s // FOX_Q_BLOCK
    cq = c.reshape(b, h, nc, FOX_Q_BLOCK).transpose(2, 0, 1, 3)
    out = lax.map(chunk, (jnp.arange(nc), to_chunks(q, FOX_Q_BLOCK), cq))
    return from_chunks(out)


def setup_inputs(seed: int = 0) -> dict:
    key = jax.random.key(seed)
    ks = jax.random.split(key, 11)
    f32 = jnp.float32
    def normal(k, shape, scale):
        return jax.random.normal(k, shape, f32) * scale
    return {
        "x": normal(ks[0], (BATCH, SEQ, D_MODEL), 1.0),
        "norm_mix_g": 1.0 + normal(ks[1], (D_MODEL,), 0.02),
        "w_in": normal(ks[2], (D_MODEL, D_IN), D_MODEL ** -0.5),
        "b_forget": 2.0 + normal(ks[3], (N_HEADS_FOX,), 0.5),
        "w_proj_a": normal(ks[4], (MOBA_WIDTH, D_MODEL), MOBA_WIDTH ** -0.5),
        "w_proj_b": normal(ks[5], (FOX_WIDTH, D_MODEL), FOX_WIDTH ** -0.5),
        "w_out": normal(ks[6], (D_MODEL, D_MODEL), D_MODEL ** -0.5),
        "norm_mlp_g": 1.0 + normal(ks[7], (D_MODEL,), 0.02),
        "w_up": normal(ks[8], (D_MODEL, D_FF), D_MODEL ** -0.5),
        "w_down": normal(ks[9], (D_FF, D_MODEL), D_FF ** -0.5),
        "norm_final_g": 1.0 + normal(ks[10], (D_MODEL,), 0.02),
    }


def reference(x, norm_mix_g, w_in, b_forget, w_proj_a, w_proj_b, w_out,
              norm_mlp_g, w_up, w_down, norm_final_g):
    split_idx = list(np.cumsum(IN_WIDTHS)[:-1])
    for _ in range(DEPTH):
        hn = rms_norm(x, norm_mix_g)
        z = hn @ w_in
        q_a, k_a, v_a, q_b, k_b, v_b, f_logit, g_a, g_b = jnp.split(z, split_idx, axis=-1)
        a = moba_attention(split_heads(q_a, N_HEADS_MOBA), split_heads(k_a, N_HEADS_MOBA),
                           split_heads(v_a, N_HEADS_MOBA))
        bfx = forgetting_attention(split_heads(q_b, N_HEADS_FOX), split_heads(k_b, N_HEADS_FOX),
                                   split_heads(v_b, N_HEADS_FOX), f_logit, b_forget)
        a = merge_heads(a) @ w_proj_a
        bfx = merge_heads(bfx) @ w_proj_b
        merged = jax.nn.sigmoid(g_a) * a + jax.nn.sigmoid(g_b) * bfx
        x = x + merged @ w_out
        hm = rms_norm(x, norm_mlp_g)
        x = x + jnp.square(jax.nn.relu(hm @ w_up)) @ w_down
    return rms_norm(x, norm_final_g)
```

```python
import os
import numpy as np
import concourse.bass as bass
import concourse.mybir as mybir
from concourse.bass_utils import run_bass_kernel_spmd

F32 = mybir.dt.float32
BF16 = mybir.dt.bfloat16
U8 = mybir.dt.uint8
AF = mybir.ActivationFunctionType
ALU = mybir.AluOpType
AX = mybir.AxisListType

NCORES = 8
S = 8192
D = 2048
DH = 128
KC = D // 128
TOK = S // NCORES
DFF = 4 * D
EPS = 1e-6
SCALE = DH ** -0.5
BIG = 30000.0
NT = S // 128
NQT = S // 512
WQ = 6 * 128 + 1
SPLITS = [(0, 5120), (5120, 7168), (7168, 7680), (7680, 8192)]


def own_rows(c):
    rows = []
    for a, b in SPLITS:
        w = (b - a) // NCORES
        rows.append(np.arange(a + c * w, a + (c + 1) * w))
    return np.concatenate(rows)

STAGE = int(os.environ.get("MK_STAGE", "9"))


class Sched:
    ENGS = ("pe", "act", "dve", "pool", "sp")

    def __init__(self):
        self.streams = {e: [] for e in self.ENGS}
        self.cnt = {e: 0 for e in self.ENGS}
        self.dcnt = {}

    def op(self, eng, fn, deps=(), signal=True):
        deps = [d for d in deps if d is not None]
        h = None
        if signal:
            self.cnt[eng] += 1
            h = ("c", eng, self.cnt[eng])
        self.streams[eng].append((fn, deps, signal, None, 0))
        return h

    def dma(self, eng, fn, sem, deps=(), inc=16):
        deps = [d for d in deps if d is not None]
        self.dcnt[sem] = self.dcnt.get(sem, 0) + inc
        h = ("d", sem, self.dcnt[sem])
        self.streams[eng].append((fn, deps, False, sem, inc))
        return h

    def wait(self, eng, deps):
        deps = [d for d in deps if d is not None]
        self.streams[eng].append((None, deps, False, None, 0))


def build_nc(stage=STAGE):
    nc = bass.Bass("TRN2", target_bir_lowering=False)
    sc = Sched()

    def din(name, shape, dt=F32):
        return nc.dram_tensor(name, list(shape), dt, kind="ExternalInput")

    x_d = din("x", [S, D])
    xo_d = din("x_own", [TOK, D])
    wqkv_d = din("w_qkv", [D, WQ])
    wg_d = din("w_gate", [D, 2 * D])
    wpa_d = din("w_proj_a", [1024, D])
    wpb_d = din("w_proj_b", [1024, D])
    wo_d = din("w_out", [D, D])
    wu_d = din("w_up", [D, DFF])
    wd_d = din("w_down", [DFF, D])
    g1_d = din("g_mix", [1, D])
    g2_d = din("g_mlp", [1, D])
    g3_d = din("g_final", [1, D])
    bf_d = din("b_for", [1, 1])
    cidb_d = din("c_identb", [128, 128], BF16)
    cidf_d = din("c_identf", [128, 128])
    ctriu_d = din("c_triu", [128, 128])
    csu_d = din("c_su", [64, 64])
    ctri_d = din("c_tri", [128, 128])
    ce_d = din("c_e", [36, 32 * 128], BF16)
    crowm_d = din("c_rowm", [NQT * 4, 512], BF16)
    coh_d = din("c_oh", [4, 512])
    ctrib_d = din("c_trib", [128, 128], BF16)
    ckps_d = din("c_kps", [128, NT])
    cqoff_d = din("c_qoff", [128, NQT])
    caql_d = din("c_aql", [1, 512])
    y_d = nc.dram_tensor("y", [TOK, D], F32, kind="ExternalOutput")
    agin_l = [nc.dram_tensor(f"ag_in{i}", [256, SPLITS[i][1] - SPLITS[i][0]], BF16, kind="Internal")
              for i in range(len(SPLITS))]
    agout_l = [nc.dram_tensor(f"ag_out{i}", [NCORES * 256, SPLITS[i][1] - SPLITS[i][0]], BF16, kind="Internal")
               for i in range(len(SPLITS))]
    cdram_d = nc.dram_tensor("c_scr", [64, 128], F32, kind="Internal")
    dbg = {}
    if stage < 9:
        dbg_d = {}

    def bcast(handle, off, n):
        return bass.AP(handle, off, [[0, 128], [1, n]])

    arena_g = nc.sbuf_tensor("arena", [128, 212000], U8)
    arena_g.__enter__()
    base = nc.lookup_mloc("arena").addr
    cur = [0]

    def salloc(name, shape, dt, at=None):
        esz = 4 if dt == F32 else 2
        n = 1
        for s_ in shape[1:]:
            n *= s_
        nbytes = (n * esz + 31) // 32 * 32
        if at is None:
            off = cur[0]
            cur[0] += nbytes
        else:
            off = at
        assert off + nbytes <= 212000, (name, off, nbytes)
        return nc.alloc_sbuf_tensor_at(name, list(shape), dt, offset=base + off), off, nbytes

    offs = {}

    def A(name, shape, dt, at=None):
        t_, off_, _ = salloc(name, shape, dt, at)
        offs[name] = off_
        return t_

    identb = A("identb", [128, 128], BF16)
    identf = A("identf", [128, 128], F32)
    onesb = A("onesb", [128, 128], BF16)
    onesf = A("onesf", [128, 128], F32)
    triu = A("triu", [128, 128], F32)
    su64 = A("su64", [64, 64], F32)
    tri = A("tri", [128, 128], F32)
    gB = A("gB", [128, D], F32)
    ssq = A("ssq", [128, NT], F32)
    rstd = A("rstd", [128, NT], F32)
    flog = A("flog", [128, NT], F32)
    kmT = A("kmT", [128, 32], F32)
    kps = A("kps", [128, NT], F32)
    qoff = A("qoff", [128, NQT], F32)
    bfor = A("bfor", [128, 1], F32)
    ctm = A("ctm", [128, NT], F32)
    negc = A("negc", [128, NT], F32)
    sm1 = A("sm1", [128, NT], F32)
    sm2 = A("sm2", [128, NT], F32)
    sm3 = A("sm3", [128, NT], F32)
    cT = A("cT", [64, 128], F32)
    bm64 = A("bm64", [64, 64], F32)
    tot64 = A("tot64", [64, 1], F32)
    gsb = [A(f"gsb{i}", [128, 32], F32) for i in range(2)]
    mx8 = [A(f"mx8{i}", [128, 8], F32) for i in range(2)]
    selt = [A(f"selt{i}", [128, 32], F32) for i in range(2)]
    biasqt = [A(f"biasqt{i}", [128, NT], F32) for i in range(2)]
    ssq2 = A("ssq2", [128, 8], F32)
    rstd2 = A("rstd2", [128, 8], F32)
    maskTM = A("maskTM", [128, NT, 32], BF16)
    P_CONST_END = cur[0]

    qTa = A("qTa", [128, S], BF16)
    kTa = A("kTa", [128, S], BF16)
    qTb = A("qTb", [128, S], BF16)
    kTb = A("kTb", [128, S], BF16)
    Vab = A("Vab", [128, NT, 256], BF16)
    P12_END = cur[0]
    wqkv = A("wqkv", [128, KC, WQ], BF16)
    xs = [A(f"xs{i}", [128, D], F32) for i in range(3)]
    xb = [A(f"xb{i}", [128, D], BF16) for i in range(2)]
    hnT = [A(f"hnT{i}", [128, KC, 512], BF16) for i in range(2)]
    qf32 = A("qf32", [128, 512], F32)
    P1_END = cur[0]
    cur[0] = P12_END
    Esel = A("Esel", [128, 32 * 128], BF16)
    aql = A("aql", [128, 512], F32)
    tbuf = [A(f"tbuf{i}", [128, 512], F32) for i in range(4)]
    pT = [A(f"pT{i}", [128, 512], BF16) for i in range(4)]
    cqB = [A(f"cqB{i}", [128, 512], F32) for i in range(2)]
    obuf = [A(f"obuf{i}", [128, 512], BF16) for i in range(4)]
    rden = [A(f"rden{i}", [128, 512], F32) for i in range(2)]
    maskTq = [A(f"maskTq{i}", [128, 512], BF16) for i in range(2)]
    pT += [A(f"pT{i}", [128, 512], BF16) for i in range(4, 8)]
    dsum = [A(f"dsum{i}", [128, 512], F32) for i in range(2)]
    ones32 = A("ones32", [128, 128], F32)
    trib = A("trib", [128, 128], BF16)
    oh4 = A("oh4", [128, 512], F32)
    crefs = A("crefs", [128, NQT], F32)
    Rf = [A(f"Rf{i}", [128, 512], BF16) for i in range(2)]
    P2_END = cur[0]
    cur[0] = P_CONST_END
    regAB = cur[0]
    hT = A("hT", [128, KC, TOK], BF16)
    attnT = A("attnT", [128, 16, TOK], BF16)
    xres = A("xres", [128, 8, D], F32, at=regAB)
    mT = A("mT", [128, KC, TOK], BF16)
    wring = [A(f"wring{i}", [128, 8192], BF16) for i in range(3)]
    actT = [A(f"actT{i}", [128, 4, TOK], BF16) for i in range(2)]
    tmp4 = [A(f"tmp4{i}", [128, 512], F32) for i in range(4)]
    xs4 = A("xs4", [128, D], F32)
    xb4 = A("xb4", [128, D], BF16)
    P4_END = cur[0]
    assert max(P1_END, P2_END, P4_END) <= 212000, (P1_END, P2_END, P4_END)

    banks = []
    for b in range(8):
        g_ = nc.psum_tensor(f"bank{b}", [128, 512], F32)
        banks.append(g_.__enter__())

    def bank_bf(b):
        return banks[b].bitcast(BF16) if hasattr(banks[b], "bitcast") else None

    bank_last = [None] * 8

    cl = []
    PRELOAD = []
    cl.append(sc.dma("sp", lambda e: e.dma_start(out=identb[:, :], in_=cidb_d.ap()), "const"))
    cl.append(sc.dma("sp", lambda e: e.dma_start(out=identf[:, :], in_=cidf_d.ap()), "const"))
    cl.append(sc.dma("sp", lambda e: e.dma_start(out=triu[:, :], in_=ctriu_d.ap()), "const"))
    cl.append(sc.dma("sp", lambda e: e.dma_start(out=su64[:, :], in_=csu_d.ap()), "const"))
    cl.append(sc.dma("sp", lambda e: e.dma_start(out=tri[:, :], in_=ctri_d.ap()), "const"))
    cl.append(sc.dma("sp", lambda e: e.dma_start(out=kps[:, :], in_=ckps_d.ap()), "const"))
    cl.append(sc.dma("sp", lambda e: e.dma_start(out=qoff[:, :], in_=cqoff_d.ap()), "const"))
    cl.append(sc.dma("sp", lambda e: e.dma_start(out=bfor[:, :], in_=bcast(bf_d, 0, 1)), "const"))
    cl.append(sc.dma("sp", lambda e: e.dma_start(out=gB[:, :], in_=bcast(g1_d, 0, D)), "const"))
    CONST = cl[-1]
    sc.op("pool", lambda e: e.memset(onesb[:, :], 1.0))
    sc.op("pool", lambda e: e.memset(onesf[:, :], 1.0))
    sc.op("pool", lambda e: e.memset(ssq[:, :], 0.0))
    sc.op("pool", lambda e: e.memset(ssq2[:, :], 0.0))
    POOLINIT = sc.op("pool", lambda e: e.memset(kmT[:, :], 0.0))
    wq_src = wqkv_d.ap().rearrange("(k p) c -> p k c", p=128)
    wq_hs = []
    for s_ in range(5):
        c0_, c1_ = (s_ * 128, (s_ + 1) * 128) if s_ < 4 else (512, WQ)
        wq_hs.append(sc.dma("pool", lambda e, c0_=c0_, c1_=c1_: e.dma_start(
            out=wqkv[:, :, c0_:c1_], in_=wq_src[:, :, c0_:c1_]), f"wq{s_}"))

    st = {"sq": {}, "xb": {}, "tr": {}, "ev": {}}

    def norm_tile(key, src_ap, xs_t, xb_t, ssq_col, rstd_col, extra_deps=(), load_deps=(), in_sbuf=None):
        if in_sbuf is None:
            ld = sc.dma("sp", lambda e: e.dma_start(out=xs_t[:, :], in_=src_ap), "xs_" + key[0] + str(key[2]),
                        deps=list(load_deps))
            xin = xs_t[:, :]
        else:
            ld = None
            xin = in_sbuf
        sq = sc.op("act", lambda e: e.activation(out=xb_t[:, :], in_=xin, func=AF.Square,
                                                 accum_out=ssq_col),
                   deps=[ld, POOLINIT] + list(extra_deps))
        r0 = sc.op("dve", lambda e: e.tensor_scalar(out=rstd_col, in0=ssq_col, scalar1=1.0 / D,
                                                    scalar2=EPS, op0=ALU.mult, op1=ALU.add), deps=[sq])
        r1 = sc.op("act", lambda e: e.activation(out=rstd_col, in_=rstd_col, func=AF.Sqrt), deps=[r0])
        r2 = sc.op("dve", lambda e: e.reciprocal(out=rstd_col, in_=rstd_col), deps=[r1])
        return sq, r2, xin

    tp_banks = [(0, 1), (2, 3)]
    pj_banks = [4, 5, 6]
    pj_i = [0]
    GPB = 7
    hn_readers = [None, None]
    xs_free = [None, None, None]
    xb_free = [None, None]
    ev_last = {}
    gate_state = {}

    p1_ld = {}

    def p1_load(i):
        s3 = i % 3
        p1_ld[i] = sc.dma("sp", lambda e: e.dma_start(out=xs[s3][:, :], in_=x_d.ap()[i * 128:(i + 1) * 128, :]),
                          f"xs_a{s3}", deps=[xs_free[s3]])

    def p1_A(i):
        sl = i % 2
        s3 = i % 3
        if i + 2 < NT:
            p1_load(i + 2)
        sq, r2, xin = norm_tile(("a", i, sl), None, None, xb[sl],
                                ssq[:, i:i + 1], rstd[:, i:i + 1],
                                extra_deps=[xb_free[sl], p1_ld[i]], in_sbuf=xs[s3][:, :])
        h = sc.op("dve", lambda e: e.scalar_tensor_tensor(out=xb[sl][:, :], in0=xs[s3][:, :],
                                                          scalar=rstd[:, i:i + 1], in1=gB[:, :],
                                                          op0=ALU.mult, op1=ALU.mult),
                  deps=[r2, CONST, sq])
        xs_free[s3] = h
        st["xb"][i] = h

    def transposes(src_xb, bankpair, dep, dst_fn, evdeps):
        hs = []
        for half in range(2):
            b = bankpair[half]
            pb = banks[b].bitcast(BF16)
            last = None
            for kk in range(8):
                k = half * 8 + kk
                last = sc.op("pe", lambda e, pb=pb, kk=kk, k=k: e.transpose(
                    out=pb[:, kk * 128:(kk + 1) * 128], in_=src_xb[:, k * 128:(k + 1) * 128],
                    identity=identb[:, :]),
                    deps=[dep, CONST, bank_last[b]], signal=(kk == 7))
            src = pb[:, :].rearrange("p (k t) -> p k t", t=128)
            if half == 0:
                ev = sc.op("act", lambda e, src=src: e.activation(out=dst_fn(0), in_=src, func=AF.Copy),
                           deps=[last] + list(evdeps))
            else:
                ev = sc.op("dve", lambda e, src=src: e.tensor_copy(out=dst_fn(1), in_=src),
                           deps=[last] + list(evdeps))
            bank_last[b] = ev
            hs.append((last, ev))
        return hs

    def p1_B(i):
        sl = i % 2
        g, sub = i // 4, i % 4
        hs = transposes(xb[sl], tp_banks[sl], st["xb"][i],
                        lambda half: hnT[g % 2][:, half * 8:(half + 1) * 8, sub * 128:(sub + 1) * 128],
                        [hn_readers[g % 2]])
        xb_free[sl] = hs[1][0]
        ev_last[i] = [hs[0][1], hs[1][1]]

    def next_pj():
        b = pj_banks[pj_i[0] % 3]
        pj_i[0] += 1
        return b

    def p1_C(g, s):
        hsl = hnT[g % 2]
        evd = ev_last[4 * g + 3] + ev_last[4 * g + 2] + ev_last[4 * g + 1] + ev_last[4 * g]
        b = next_pj()
        last = None
        for k in range(KC):
            last = sc.op("pe", lambda e, b=b, k=k: e.matmul(
                banks[b][:, :], lhsT=wqkv[:, k, s * 128:(s + 1) * 128], rhs=hsl[:, k, :],
                start=(k == 0), stop=(k == KC - 1)),
                deps=evd + [wq_hs[s], bank_last[b]], signal=(k == KC - 1))
        cols = slice(g * 512, (g + 1) * 512)
        if s == 0:
            e1 = sc.op("act", lambda e, b=b: e.activation(out=qf32[:, :], in_=banks[b][:, :], func=AF.Copy),
                       deps=[last, gate_state.get("qf_free"), gate_state.get("qf_rd")])
            e2 = sc.op("dve", lambda e: e.tensor_copy(out=qTa[:, cols], in_=qf32[:, :]), deps=[e1])
            bank_last[b] = e1
            gate_state["qf"] = e1
            gate_state["qf_rd"] = e2
        elif s == 1:
            ee = None
            for h2 in range(2):
                ee = sc.op("act", lambda e, b=b, h2=h2: e.activation(
                    out=kTa[:, g * 512 + h2 * 256: g * 512 + (h2 + 1) * 256],
                    in_=banks[b][:, h2 * 256:(h2 + 1) * 256], func=AF.Copy,
                    accum_out=kmT[:, 2 * g + h2: 2 * g + h2 + 1]), deps=[last, POOLINIT])
            bank_last[b] = ee
            gate_state["km"] = ee
        elif s == 2:
            ee = sc.op("dve", lambda e, b=b: e.tensor_copy(out=qTb[:, cols], in_=banks[b][:, :]), deps=[last])
            bank_last[b] = ee
        else:
            ee = sc.op("act", lambda e, b=b: e.activation(out=kTb[:, cols], in_=banks[b][:, :], func=AF.Copy),
                       deps=[last])
            bank_last[b] = ee
        b = next_pj()
        i = 4 * g + s
        last = None
        for k in range(KC):
            last = sc.op("pe", lambda e, b=b, k=k: e.matmul(
                banks[b][:, 0:257], lhsT=hsl[:, k, s * 128:(s + 1) * 128], rhs=wqkv[:, k, 512:769],
                start=(k == 0), stop=(k == KC - 1)),
                deps=evd + [wq_hs[4], bank_last[b]], signal=(k == KC - 1))
        if s == 3:
            hn_readers[g % 2] = last
        e1 = sc.op("dve", lambda e, b=b: e.tensor_copy(out=Vab[:, i, :], in_=banks[b][:, 0:256]), deps=[last])
        e2 = sc.op("act", lambda e, b=b: e.activation(out=flog[:, i:i + 1], in_=banks[b][:, 256:257],
                                                      func=AF.Copy), deps=[last, e1])
        bank_last[b] = e2
        if s == 1:
            glast = None
            for u in range(4):
                ti = 4 * g + u
                blk = ti // 2
                gs = gsb[u % 2]
                m8 = mx8[u % 2]
                se = selt[u % 2]
                d0 = sc.op("pool", lambda e, gs=gs: e.memset(gs[:, :], -BIG), deps=[gate_state.get(("gsrd", u % 2))])
                if blk > 0:
                    gm = sc.op("pe", lambda e, u=u, blk=blk: e.matmul(
                        banks[GPB][:, u * 32:u * 32 + blk], lhsT=qf32[:, u * 128:(u + 1) * 128],
                        rhs=kmT[:, 0:blk], start=True, stop=True),
                        deps=[gate_state["qf"], gate_state["km"], bank_last[GPB]])
                    glast = gm
                    d0 = sc.op("dve", lambda e, gs=gs, u=u, blk=blk: e.tensor_copy(
                        out=gs[:, 0:blk], in_=banks[GPB][:, u * 32:u * 32 + blk]), deps=[gm, d0])
                    gate_state["gp_rd"] = d0
                    bank_last[GPB] = d0
                d1 = sc.op("dve", lambda e, gs=gs, m8=m8: e.max(out=m8[:, :], in_=gs[:, :]), deps=[d0])
                d2 = sc.op("dve", lambda e, m8=m8: e.tensor_scalar_max(out=m8[:, 2:3], in0=m8[:, 2:3],
                                                                        scalar1=-BIG / 2), deps=[d1])
                d3 = sc.op("dve", lambda e, gs=gs, m8=m8, se=se: e.tensor_scalar(
                    out=se[:, :], in0=gs[:, :], scalar1=m8[:, 2:3], scalar2=None, op0=ALU.is_ge), deps=[d2])
                d4 = sc.op("dve", lambda e, se=se, blk=blk: e.memset(se[:, blk:blk + 1], 1.0), deps=[d3])
                d5 = sc.op("dve", lambda e, se=se, ti=ti: e.tensor_scalar(
                    out=maskTM[:, ti, :], in0=se[:, :], scalar1=1.0, scalar2=BIG,
                    op0=ALU.subtract, op1=ALU.mult), deps=[d4])
                gate_state[("gsrd", u % 2)] = d5
            if glast is not None:
                gate_state["qf_free"] = glast

    NT1 = NT if stage >= 1 else 0
    if NT1:
        n_sp0 = len(sc.streams["sp"])
        p1_load(0)
        p1_load(1)
        first2 = sc.streams["sp"][n_sp0:n_sp0 + 2]
        del sc.streams["sp"][n_sp0:n_sp0 + 2]
        sc.streams["sp"][0:0] = first2
        p1_A(0)
        for i in range(NT1):
            if i + 1 < NT1:
                p1_A(i + 1)
            p1_B(i)
            if i >= 4:
                p1_C(i // 4 - 1, i % 4)
        for s in range(4):
            p1_C(NT1 // 4 - 1, s)

    final_waits = []
    ob_handles = []
    if stage >= 2:
        P1DONE_PE = ("c", "pe", sc.cnt["pe"])
        P1DONE_ACT = ("c", "act", sc.cnt["act"])
        P1DONE_DVE = ("c", "dve", sc.cnt["dve"])
        p1done = [P1DONE_PE, P1DONE_ACT, P1DONE_DVE]
        sc.op("pool", lambda e: e.memset(ones32[:, :], 1.0 / 32.0), deps=p1done)
        sc.op("pool", lambda e: e.memset(Esel[:, :], 0.0), deps=p1done)
        sc.op("pool", lambda e: e.memset(maskTq[0][:, :], 0.0), deps=p1done)
        sc.op("pool", lambda e: e.memset(maskTq[1][:, :], 0.0), deps=p1done)
        PAD0 = ("c", "pool", sc.cnt["pool"])
        c2 = sc.dma("sp", lambda e: e.dma_start(out=Esel[0:36, :], in_=ce_d.ap()), "const2", deps=p1done + [PAD0])
        c2 = sc.dma("sp", lambda e: e.dma_start(out=trib[:, :], in_=ctrib_d.ap()), "const2", deps=p1done)
        c2 = sc.dma("sp", lambda e: e.dma_start(out=oh4[32:36, :], in_=coh_d.ap()), "const2", deps=p1done)
        c2 = sc.dma("sp", lambda e: e.dma_start(out=aql[:, :], in_=bcast(caql_d, 0, 512)), "const2", deps=p1done)
        CONST2 = c2
        for b in range(8):
            bank_last[b] = None

        z = sc.op("dve", lambda e: e.tensor_scalar(out=sm1[:, :], in0=flog[:, :], scalar1=bfor[:, 0:1],
                                                   scalar2=None, op0=ALU.add), deps=p1done + [CONST])
        az = sc.op("act", lambda e: e.activation(out=sm2[:, :], in_=sm1[:, :], func=AF.Abs), deps=[z] + p1done)
        ex = sc.op("act", lambda e: e.activation(out=sm2[:, :], in_=sm2[:, :], func=AF.Exp, scale=-1.0),
                   deps=[az] + p1done)
        ln = sc.op("act", lambda e: e.activation(out=sm2[:, :], in_=sm2[:, :], func=AF.Ln, bias=1.0),
                   deps=[ex])
        mn = sc.op("dve", lambda e: e.tensor_scalar_min(out=sm1[:, :], in0=sm1[:, :], scalar1=0.0), deps=[z, ln])
        lf = sc.op("dve", lambda e: e.tensor_sub(out=sm3[:, :], in0=sm1[:, :], in1=sm2[:, :]), deps=[mn, ln])
        t1 = sc.op("pe", lambda e: e.transpose(out=banks[0][0:64, 0:128], in_=sm3[:, :], identity=identf[:, :]),
                   deps=[lf] + p1done)
        tt_ = sc.op("dve", lambda e: e.reduce_sum(out=tot64[:, :], in_=banks[0][0:64, 0:128], axis=AX.X), deps=[t1])
        bmh = sc.op("dve", lambda e: e.tensor_scalar(out=bm64[:, :], in0=su64[:, :], scalar1=tot64[:, 0:1],
                                                     scalar2=None, op0=ALU.mult), deps=[tt_])
        sc.op("pe", lambda e: e.matmul(banks[1][:, 0:64], lhsT=triu[:, :], rhs=sm3[:, :], start=True, stop=False),
              deps=[lf], signal=False)
        cm = sc.op("pe", lambda e: e.matmul(banks[1][:, 0:64], lhsT=onesf[0:64, :], rhs=bm64[:, :],
                                            start=False, stop=True), deps=[bmh, POOLINIT])
        cc = sc.op("dve", lambda e: e.tensor_copy(out=ctm[:, :], in_=banks[1][:, 0:64]), deps=[cm])
        ng = sc.op("dve", lambda e: e.tensor_scalar_mul(out=negc[:, :], in0=ctm[:, :], scalar1=-1.0), deps=[cc])
        t2 = sc.op("pe", lambda e: e.transpose(out=banks[0][0:64, 0:128], in_=ctm[:, :], identity=identf[:, :]),
                   deps=[cc, tt_])
        ct_h = sc.op("dve", lambda e: e.tensor_copy(out=cT[:, :], in_=banks[0][0:64, 0:128]), deps=[t2])
        cst = sc.dma("sp", lambda e: e.dma_start(out=cdram_d.ap(), in_=cT[:, :]), "cst", deps=[ct_h])
        crf = sc.dma("sp", lambda e: e.dma_start(out=crefs[32:36, :],
                                                 in_=bass.AP(cdram_d, 127, [[128, 4], [512, NQT]]),
                                                 allow_slow_non_contiguous=True),
                     "crf", deps=[cst])
        bank_last[0] = ct_h
        bank_last[1] = ng
        dbg["ctm"] = (ctm, [128, NT], F32, [ng])

        SBK = [0, 1, 2, 3]
        OB = {0: 4, 1: 6}
        DB = {0: 5, 1: 7}
        sb_i = [0]
        t_rd = [None] * 4
        p_rd = [None] * 8
        pend = {0: [], 1: []}
        dsum_free = [None, None]
        cq_rd = [None, None]
        mq_rd = [None, None]
        bq_rd = [None, None]
        ob_free = [None] * 4
        rd_free = [None, None]
        ring_i = [0]

        sb_owner = {b_: None for b_ in SBK}

        def take_sb():
            for t_ in range(4):
                b_ = SBK[(sb_i[0] + t_) % 4]
                if sb_owner[b_] is None:
                    sb_i[0] = (sb_i[0] + t_ + 1) % 4
                    sb_owner[b_] = 1
                    return b_
            raise AssertionError("no free S^T bank")

        def tiles_for(typ, QT):
            return [(typ, QT, j) for j in range(4 * QT + 4)]

        def qt_prologue(typ, QT):
            sl = QT % 2
            if typ == 0:
                h = sc.op("pool", lambda e: e.tensor_scalar(out=Rf[sl][32:36, :], in0=oh4[32:36, :],
                                                            scalar1=crefs[32:36, QT:QT + 1], scalar2=1.0 / SCALE,
                                                            op0=ALU.mult, op1=ALU.mult),
                          deps=[crf, CONST2, cq_rd[sl]])
                return {"rf": Rf[sl], "rfh": h, "bias": negc, "biash": ng}
            b = take_sb()
            sb_owner[b] = None
            pb = banks[b].bitcast(BF16)
            last = None
            for u in range(4):
                last = sc.op("pe", lambda e, u=u, pb=pb: e.transpose(
                    out=pb[0:32, u * 128:(u + 1) * 128], in_=maskTM[:, 4 * QT + u, :], identity=identb[:, :]),
                    deps=[bank_last[b]] + p1done, signal=(u == 3))
            mh = sc.op("act", lambda e, pb=pb: e.activation(out=maskTq[sl][0:32, :], in_=pb[0:32, 0:512], func=AF.Copy),
                       deps=[last, mq_rd[sl], PAD0])
            bank_last[b] = mh
            rh = sc.dma("sp", lambda e: e.dma_start(out=maskTq[sl][32:36, :], in_=crowm_d.ap()[4 * QT:4 * QT + 4, :]),
                        f"rowm{sl}", deps=[mq_rd[sl], PAD0])
            return {"bias": kps, "biash": CONST, "mq": maskTq[sl], "mqh": mh, "rmh": rh}

        def qk(tile, ctx):
            typ, QT, j = tile
            b = take_sb()
            jj = j - 4 * QT
            q0 = 128 * jj if jj > 0 else 0
            diag = jj >= 0
            kT_ = kTb if typ == 0 else kTa
            qT_ = qTb if typ == 0 else qTa
            sc.op("pe", lambda e: e.matmul(banks[b][:, q0:512], lhsT=kT_[:, j * 128:(j + 1) * 128],
                                           rhs=qT_[:, QT * 512 + q0:(QT + 1) * 512],
                                           start=True, stop=False),
                  deps=[bank_last[b]] + p1done, signal=False)
            if typ == 0:
                h = sc.op("pe", lambda e: e.matmul(banks[b][:, q0:512], lhsT=Esel[32:36, 0:128],
                                                   rhs=ctx["rf"][32:36, q0:512], start=False, stop=not diag),
                          deps=[ctx["rfh"], CONST2, PAD0], signal=not diag)
            else:
                n = j // 2
                h = sc.op("pe", lambda e: e.matmul(banks[b][:, q0:512], lhsT=Esel[:, n * 128:(n + 1) * 128],
                                                   rhs=ctx["mq"][:, q0:512], start=False, stop=not diag),
                          deps=[ctx["mqh"], ctx["rmh"], CONST2, PAD0], signal=not diag)
            if diag:
                h = sc.op("pe", lambda e: e.matmul(banks[b][:, q0:q0 + 128], lhsT=identb[:, :], rhs=trib[:, :],
                                                   start=False, stop=True), deps=[CONST, CONST2])
            return {"b": b, "q0": q0, "jj": jj, "qk": h}

        def rest(tile, ctx, info, first, last_):
            typ, QT, j = tile
            b, q0, jj = info["b"], info["q0"], info["jj"]
            r = ring_i[0] % 4
            rp = ring_i[0] % 8
            ring_i[0] += 1
            pt = pT[rp]
            h2 = sc.op("act", lambda e: e.activation(out=pt[:, q0:512], in_=banks[b][:, q0:512], func=AF.Exp,
                                                     bias=ctx["bias"][:, j:j + 1], scale=SCALE),
                       deps=[info["qk"], ctx["biash"], p_rd[rp]])
            bank_last[b] = h2
            sb_owner[b] = None
            ctx["bias_rd"] = h2
            vsl = slice(128, 256) if typ == 0 else slice(0, 128)
            sc.op("pe", lambda e: e.matmul(banks[OB[typ]][:, q0:512], lhsT=Vab[:, j, vsl], rhs=pt[:, q0:512],
                                           start=first, stop=last_),
                  deps=[h2, bank_last[OB[typ]] if first else None], signal=False)
            if QT == 0:
                h3 = sc.op("pe", lambda e: e.matmul(banks[DB[typ]][:, q0:512], lhsT=onesb[:, :], rhs=pt[:, q0:512],
                                                    start=first, stop=last_),
                           deps=[h2, POOLINIT, bank_last[DB[typ]] if first else None])
                p_rd[rp] = h3
                return h3
            pend[typ].append((pt, q0, rp, h2, j))
            h3 = None
            if j % 4 == 3:
                for g_, (pt_, q0_, rp_, h2_, j_) in enumerate(pend[typ]):
                    h3 = sc.op("pe", lambda e, g_=g_, pt_=pt_, q0_=q0_, j_=j_: e.matmul(
                        banks[DB[typ]][32 * g_:32 * g_ + 32, q0_:512], lhsT=onesb[:, 32 * g_:32 * g_ + 32],
                        rhs=pt_[:, q0_:512], start=(j_ < 4), stop=(j_ >= 4 * QT),
                        tile_position=(0, 32 * g_), skip_group_check=True),
                        deps=[h2_, POOLINIT, bank_last[DB[typ]] if j_ < 4 else None], signal=(g_ == 3))
                for (_, _, rp_, _, _) in pend[typ]:
                    p_rd[rp_] = h3
                pend[typ] = []
            return h3

        oi = [0]
        seg_done = {}
        ag_h = []

        def qt_epilogue(typ, QT, ctx, lastpv):
            sl = typ
            o = oi[0] % 4
            oi[0] += 1
            if QT > 0:
                c1 = sc.op("act", lambda e: e.activation(out=dsum[sl][:, :], in_=banks[DB[typ]][:, :], func=AF.Copy),
                           deps=[lastpv, dsum_free[sl]])
                lastpv = sc.op("pe", lambda e: e.matmul(banks[DB[typ]][:, :], lhsT=ones32[:, :], rhs=dsum[sl][:, :],
                                                        start=True, stop=True), deps=[c1, PAD0])
                dsum_free[sl] = lastpv
            r1 = sc.op("dve", lambda e: e.reciprocal(out=rden[sl][:, :], in_=banks[DB[typ]][:, :]),
                       deps=[lastpv, rd_free[sl]])
            r2 = sc.op("dve", lambda e: e.tensor_tensor(out=obuf[o][:, :], in0=banks[OB[typ]][:, :],
                                                        in1=rden[sl][:, :], op=ALU.mult),
                       deps=[r1, ob_free[o]])
            rd_free[sl] = r2
            bank_last[DB[typ]] = r1
            bank_last[OB[typ]] = r2
            row0 = 0 if typ == 1 else 128
            si = [i for i, (a_, b_) in enumerate(SPLITS) if a_ <= QT * 512 < b_][0]
            c0 = QT * 512 - SPLITS[si][0]
            dh = sc.dma("sp", lambda e: e.dma_start(out=agin_l[si].ap()[row0:row0 + 128, c0:c0 + 512],
                                                    in_=obuf[o][:, :]), f"ob{o}", deps=[r2])
            ob_free[o] = dh
            ob_handles.append(dh)
            seg_done[(typ, QT)] = True
            if (QT + 1) * 512 == SPLITS[si][1] and seg_done.get((1 - typ, QT)):
                ag_h.append(sc.dma("pool", lambda e: e.collective_compute(
                    "AllGather", ALU.bypass, replica_groups=[list(range(NCORES))],
                    ins=[agin_l[si].ap()], outs=[agout_l[si].ap()]), "cc",
                    deps=[h_ for h_ in ob_free if h_ is not None], inc=1))
            if typ == 0:
                cq_rd[QT % 2] = ctx["last_qk"]
            else:
                mq_rd[QT % 2] = ctx["last_qk"]
                bq_rd[QT % 2] = ctx["bias_rd"]

        seq = []
        nqt = NQT if stage >= 3 else 2
        for QT in range(nqt):
            for typ in (0, 1):
                tl = tiles_for(typ, QT)
                for idx, t in enumerate(tl):
                    seq.append((t, idx == 0, idx == len(tl) - 1))
        LOOK = 3
        HOIST = 6
        ctxs = {}
        infos = {}

        def maybe_prologue(k, force):
            if k >= len(seq):
                return
            tile, first, _ = seq[k]
            key = (tile[0], tile[1])
            if not first or key in ctxs:
                return
            if force or tile[1] == 0 or seg_done.get((tile[0], tile[1] - 1)):
                ctxs[key] = qt_prologue(tile[0], tile[1])

        for n in range(len(seq) + LOOK):
            if n < len(seq):
                for k_ in range(n + 1, n + HOIST + 1):
                    maybe_prologue(k_, False)
                maybe_prologue(n, True)
                tile, first, last_ = seq[n]
                ctx = ctxs[(tile[0], tile[1])]
                infos[n] = qk(tile, ctx)
                ctx["last_qk"] = infos[n]["qk"]
            m = n - LOOK
            if m >= 0:
                tile, first, last_ = seq[m]
                ctx = ctxs[(tile[0], tile[1])]
                h3 = rest(tile, ctx, infos[m], first, last_)
                if last_:
                    qt_epilogue(tile[0], tile[1], ctx, h3)
        dbg["maskTM"] = (maskTM, [128, NT * 32], BF16, [("c", "dve", sc.cnt["dve"])])

    if stage >= 4:
        P2DONE = [("c", "pe", sc.cnt["pe"]), ("c", "act", sc.cnt["act"]), ("c", "dve", sc.cnt["dve"]),
                  ("c", "pool", sc.cnt["pool"])]
        at_h = None
        col = 0
        for si, (a_, b_) in enumerate(SPLITS):
            w = (b_ - a_) // NCORES

            def ld_attn(e, si=si, w=w, col=col):
                pid = e.partition_id()
                src_ = agout_l[si].ap().rearrange("(rt p) c -> p rt c", p=128)[:, :, bass.ds(pid * w, w)]
                return e.dma_start(out=attnT[:, :, col:col + w], in_=src_)
            at_h = sc.dma("pool", ld_attn, "attn", deps=[ag_h[si]] + P2DONE)
            col += w

        for b in range(8):
            bank_last[b] = None
        g4 = P2DONE
        xs4b = A("xs4b", [128, D], F32, at=offs["tmp40"])
        xb4b = A("xb4b", [128, D], BF16, at=offs["actT0"])
        xs4l, xb4l = [xs4, xs4b], [xb4, xb4b]
        xs4_fr = [None, None]
        xb4_fr = [None, None]
        ev4 = []
        h = None
        for ts in range(8):
            sl4 = ts % 2
            sq, r2, xin = norm_tile(("o", ts, sl4), xo_d.ap()[ts * 128:(ts + 1) * 128, :], xs4l[sl4], xb4l[sl4],
                                    ssq2[:, ts:ts + 1], rstd2[:, ts:ts + 1],
                                    extra_deps=[xb4_fr[sl4]] + g4, load_deps=[xs4_fr[sl4]] + g4)
            h = sc.op("dve", lambda e, ts=ts, sl4=sl4: e.scalar_tensor_tensor(
                out=xb4l[sl4][:, :], in0=xs4l[sl4][:, :], scalar=rstd2[:, ts:ts + 1], in1=gB[:, :],
                op0=ALU.mult, op1=ALU.mult), deps=[r2, sq] + g4)
            xs4_fr[sl4] = h
            hs = transposes(xb4l[sl4], tp_banks[sl4], h,
                            lambda half, ts=ts: hT[:, half * 8:(half + 1) * 8, ts * 128:(ts + 1) * 128], g4)
            xb4_fr[sl4] = hs[1][0]
            ev4 += [hs[0][1], hs[1][1]]
        xs4_free = h
        xb4_free = xb4_fr[0]
        A4_PE = hs[1][0]
        HT_READY = ev4[-2:]
        GB1_FREE = xs4_free


        wr_free = [None, None, None]
        wr_i = [0]

        def wload(parts):
            s_ = wr_i[0] % 3
            wr_i[0] += 1
            wt = wring[s_]
            h = None
            for dst_fn, src in parts:
                h = sc.dma("pool", lambda e, dst_fn=dst_fn, src=src, wt=wt: e.dma_start(out=dst_fn(wt), in_=src),
                           f"wr{s_}", deps=[wr_free[s_]] + g4)
            return s_, wt, h

        def v3(wt, nk, nc_, off=0):
            return wt[:, off:off + nk * nc_].rearrange("p (k c) -> p k c", c=nc_)

        pbank_i = [0]

        def nb():
            b = pbank_i[0] % 8
            pbank_i[0] += 1
            return b

        items = []
        S4 = {"mt_last": None, "xacc": None, "xb4_free": xb4_free, "gbfree": GB1_FREE}
        tmp_free = [None] * 4
        wg_src = wg_d.ap().rearrange("(k p) c -> p k c", p=128)
        wpa_src = wpa_d.ap().rearrange("(k p) c -> p k c", p=128)
        wpb_src = wpb_d.ap().rearrange("(k p) c -> p k c", p=128)
        wo_src = wo_d.ap().rearrange("(k p) c -> p k c", p=128)
        wu_src = wu_d.ap().rearrange("(k p) c -> p k c", p=128)
        wd_src = wd_d.ap().rearrange("(f p) c -> p f c", p=128)

        def parts_b(dc):
            c0 = dc * 128
            return [(lambda wt: v3(wt, 16, 128, 0), wg_src[:, :, c0:c0 + 128]),
                    (lambda wt: v3(wt, 16, 128, 2048), wg_src[:, :, D + c0:D + c0 + 128]),
                    (lambda wt: v3(wt, 8, 128, 4096), wpa_src[:, :, c0:c0 + 128]),
                    (lambda wt: v3(wt, 8, 128, 5120), wpb_src[:, :, c0:c0 + 128])]

        def cons_b(dc):
            def f(wt, hw):
                lastpe = None
                for tt in range(2):
                    tk = slice(tt * 512, (tt + 1) * 512)
                    bks = [nb() for _ in range(4)]
                    hh = []
                    for m in range(2):
                        wv = v3(wt, 16, 128, m * 2048)
                        b = bks[m]
                        l_ = None
                        for k in range(KC):
                            l_ = sc.op("pe", lambda e, b=b, k=k, wv=wv, tk=tk: e.matmul(
                                banks[b][:, :], lhsT=wv[:, k, :], rhs=hT[:, k, tk],
                                start=(k == 0), stop=(k == KC - 1)),
                                deps=[hw, bank_last[b]] + HT_READY, signal=(k == KC - 1))
                        hh.append(l_)
                    for m in range(2):
                        wv = v3(wt, 8, 128, 4096 + m * 1024)
                        b = bks[2 + m]
                        l_ = None
                        for h_ in range(8):
                            l_ = sc.op("pe", lambda e, b=b, h_=h_, wv=wv, m=m, tk=tk: e.matmul(
                                banks[b][:, :], lhsT=wv[:, h_, :],
                                rhs=attnT[:, 2 * h_ + m, tk], start=(h_ == 0), stop=(h_ == 7)),
                                deps=[hw, at_h, bank_last[b]], signal=(h_ == 7))
                        hh.append(l_)
                        lastpe = l_
                    sg = []
                    for m in range(2):
                        s1 = sc.op("act", lambda e, m=m, b=bks[m]: e.activation(
                            out=tmp4[m][:, :], in_=banks[b][:, :], func=AF.Sigmoid),
                            deps=[hh[m], tmp_free[m]])
                        bank_last[bks[m]] = s1
                        sg.append(s1)
                    ml = []
                    for m in range(2):
                        m1 = sc.op("dve", lambda e, m=m, b=bks[2 + m]: e.tensor_tensor(
                            out=tmp4[2 + m][:, :], in0=banks[b][:, :], in1=tmp4[m][:, :], op=ALU.mult),
                            deps=[hh[2 + m], sg[m], tmp_free[2 + m]])
                        bank_last[bks[2 + m]] = m1
                        tmp_free[m] = m1
                        ml.append(m1)
                    S4["mt_last"] = sc.op("pool", lambda e, tk=tk: e.tensor_tensor(
                        out=mT[:, dc, tk], in0=tmp4[2][:, :], in1=tmp4[3][:, :], op=ALU.add), deps=ml)
                    tmp_free[2] = S4["mt_last"]
                    tmp_free[3] = S4["mt_last"]
                return lastpe
            return f

        if stage >= 5:
            for dc in range(KC):
                items.append((parts_b(dc), cons_b(dc)))

        def parts_o(ct):
            return [(lambda wt, q=q: v3(wt, 16, 512)[:, q * 4:(q + 1) * 4, :],
                     wo_src[:, q * 4:(q + 1) * 4, ct * 512:(ct + 1) * 512]) for q in range(4)]

        def cons_o(ct):
            def f(wt, hw):
                if ct == 0:
                    P4B = [("c", "pe", sc.cnt["pe"]), ("c", "pool", sc.cnt["pool"]), ("c", "dve", sc.cnt["dve"]),
                           ("c", "act", sc.cnt["act"])]
                    for ts in range(8):
                        S4["xr_h"] = sc.dma("sp", lambda e, ts=ts: e.dma_start(
                            out=xres[:, ts, :], in_=xo_d.ap()[ts * 128:(ts + 1) * 128, :]), "xres", deps=P4B)
                wv = v3(wt, 16, 512)
                lastpe = None
                for ts in range(8):
                    b = nb()
                    l_ = None
                    for k in range(KC):
                        l_ = sc.op("pe", lambda e, b=b, k=k, ts=ts: e.matmul(
                            banks[b][:, :], lhsT=mT[:, k, ts * 128:(ts + 1) * 128], rhs=wv[:, k, :],
                            start=(k == 0), stop=(k == KC - 1)),
                            deps=[hw, S4["mt_last"], bank_last[b]], signal=(k == KC - 1))
                    lastpe = l_
                    S4["xacc"] = sc.op("dve", lambda e, b=b, ts=ts: e.tensor_tensor(
                        out=xres[:, ts, ct * 512:(ct + 1) * 512], in0=banks[b][:, :],
                        in1=xres[:, ts, ct * 512:(ct + 1) * 512], op=ALU.add), deps=[l_, S4["xr_h"]])
                    bank_last[b] = S4["xacc"]
                    S4.setdefault("xacc_o", {})[ts] = S4["xacc"]
                    S4.setdefault("pe_o", {})[ts] = l_
                S4["wout_pe"] = lastpe
                if ct == 3 and stage >= 7:
                    phase_4d()
                return lastpe
            return f

        hmT = mT

        def phase_4d():
            gb2 = sc.dma("sp", lambda e: e.dma_start(out=gB[:, :], in_=bcast(g2_d, 0, D)), "gb2",
                         deps=[S4["gbfree"]])
            sc.op("pool", lambda e: e.memset(ssq2[:, :], 0.0), deps=[S4["gbfree"]])
            z2 = ("c", "pool", sc.cnt["pool"])
            ev5 = []
            h = None
            for ts in range(8):
                sq, r2, xin = norm_tile(("m", ts, 0), None, None, xb4, ssq2[:, ts:ts + 1], rstd2[:, ts:ts + 1],
                                        extra_deps=[S4["xb4_free"], S4["xacc_o"][ts], z2], in_sbuf=xres[:, ts, :])
                h = sc.op("dve", lambda e, ts=ts: e.scalar_tensor_tensor(out=xb4[:, :], in0=xres[:, ts, :],
                                                                         scalar=rstd2[:, ts:ts + 1], in1=gB[:, :],
                                                                         op0=ALU.mult, op1=ALU.mult),
                          deps=[r2, sq, gb2])
                hs = transposes(xb4, tp_banks[ts % 2], h,
                                lambda half, ts=ts: hmT[:, half * 8:(half + 1) * 8, ts * 128:(ts + 1) * 128],
                                [S4["pe_o"][ts]])
                S4["xb4_free"] = hs[1][0]
                ev5 += [hs[0][1], hs[1][1]]
            S4["hm_ready"] = ev5[-2:]
            S4["gbfree"] = h

        if stage >= 6:
            for ct in range(4):
                items.append((parts_o(ct), cons_o(ct)))

        act_free = [None, None]
        rl_free = [None, None]
        act_ready = {}

        def parts_u(fg):
            return [(lambda wt, q=q: v3(wt, 16, 512)[:, q * 4:(q + 1) * 4, :],
                     wu_src[:, q * 4:(q + 1) * 4, fg * 512:(fg + 1) * 512]) for q in range(4)]

        def parts_d(fg):
            return [(lambda wt, q=q: v3(wt, 4, 2048)[:, q:q + 1, :],
                     wd_src[:, fg * 4 + q:fg * 4 + q + 1, :]) for q in range(4)]

        def cons_u(fg):
            def f(wt, hw):
                wv = v3(wt, 16, 512)
                a_ = actT[fg % 2]
                lastpe = None
                sqh = None
                for fc in range(4):
                    for tt in range(2):
                        tk = slice(tt * 512, (tt + 1) * 512)
                        b = nb()
                        l_ = None
                        for k in range(KC):
                            l_ = sc.op("pe", lambda e, b=b, k=k, fc=fc, tk=tk: e.matmul(
                                banks[b][:, :], lhsT=wv[:, k, fc * 128:(fc + 1) * 128], rhs=hmT[:, k, tk],
                                start=(k == 0), stop=(k == KC - 1)),
                                deps=[hw, bank_last[b]] + S4["hm_ready"], signal=(k == KC - 1))
                        lastpe = l_
                        r = (fc * 2 + tt) % 2
                        rl = sc.op("act", lambda e, b=b, r=r: e.activation(out=tmp4[r][:, :], in_=banks[b][:, :],
                                                                           func=AF.Relu),
                                   deps=[l_, rl_free[r]])
                        bank_last[b] = rl
                        sqh = sc.op("pool", lambda e, r=r, fc=fc, tk=tk: e.tensor_tensor(
                            out=a_[:, fc, tk], in0=tmp4[r][:, :], in1=tmp4[r][:, :], op=ALU.mult),
                            deps=[rl, act_free[fg % 2]])
                        rl_free[r] = sqh
                act_ready[fg] = sqh
                return lastpe
            return f

        def cons_d(fg):
            def f(wt, hw):
                wv = v3(wt, 4, 2048)
                a_ = actT[fg % 2]
                lastpe = None
                for ts in range(8):
                    for ct in range(4):
                        b = nb()
                        l_ = None
                        for fc in range(4):
                            l_ = sc.op("pe", lambda e, b=b, fc=fc, ts=ts, ct=ct: e.matmul(
                                banks[b][:, :], lhsT=a_[:, fc, ts * 128:(ts + 1) * 128],
                                rhs=wv[:, fc, ct * 512:(ct + 1) * 512], start=(fc == 0), stop=(fc == 3)),
                                deps=[hw, act_ready[fg], bank_last[b]], signal=(fc == 3))
                        lastpe = l_
                        S4["xacc"] = sc.op("dve", lambda e, b=b, ts=ts, ct=ct: e.tensor_tensor(
                            out=xres[:, ts, ct * 512:(ct + 1) * 512], in0=banks[b][:, :],
                            in1=xres[:, ts, ct * 512:(ct + 1) * 512], op=ALU.add), deps=[l_])
                        bank_last[b] = S4["xacc"]
                        S4.setdefault("xacc_d", {})[ts] = S4["xacc"]
                act_free[fg % 2] = lastpe
                return lastpe
            return f

        if stage >= 7:
            NFG = 16 if stage >= 8 else 2
            order = [("u", 0)]
            for fg in range(1, NFG):
                order += [("u", fg), ("d", fg - 1)]
            order.append(("d", NFG - 1))
            for kind, fg in order:
                if kind == "u":
                    items.append((parts_u(fg), cons_u(fg)))
                else:
                    items.append((parts_d(fg), cons_d(fg)))

        loaded = {}
        for n in range(min(3, len(items))):
            loaded[n] = wload(items[n][0])
        for m_ in range(len(items)):
            s_, wt, hw = loaded[m_]
            lp = items[m_][1](wt, hw)
            wr_free[s_] = lp
            if m_ + 3 < len(items):
                loaded[m_ + 3] = wload(items[m_ + 3][0])

        if stage >= 7:
            gb3 = sc.dma("sp", lambda e: e.dma_start(out=gB[:, :], in_=bcast(g3_d, 0, D)), "gb3",
                         deps=[S4["gbfree"]])
            sc.op("pool", lambda e: e.memset(ssq2[:, :], 0.0), deps=[S4["gbfree"]])
            z3 = ("c", "pool", sc.cnt["pool"])
            ysl = [xs4] + [A(f"ys{i}", [128, D], F32, at=offs[f"wring{i}"]) for i in range(3)]
            ys_free = [None] * 4
            ys_wr = [None] + [wr_free[i] for i in range(3)]
            ysth = []
            for ts in range(8):
                yb = ts % 4
                sq, r2, xin = norm_tile(("f", ts, 0), None, None, xb4, ssq2[:, ts:ts + 1], rstd2[:, ts:ts + 1],
                                        extra_deps=[S4["xacc_d"][ts], z3, S4["xb4_free"]],
                                        in_sbuf=xres[:, ts, :])
                h = sc.op("dve", lambda e, ts=ts, yb=yb: e.scalar_tensor_tensor(
                    out=ysl[yb][:, :], in0=xres[:, ts, :], scalar=rstd2[:, ts:ts + 1], in1=gB[:, :],
                    op0=ALU.mult, op1=ALU.mult), deps=[r2, sq, gb3, ys_free[yb], ys_wr[yb]])
                yst = sc.dma("sp", lambda e, ts=ts, yb=yb: e.dma_start(out=y_d.ap()[ts * 128:(ts + 1) * 128, :],
                                                                       in_=ysl[yb][:, :]), f"yst{yb}", deps=[h])
                ys_free[yb] = yst
                ysth.append(yst)
            final_waits.extend(ysth[-4:])
        dbg["x"] = 1

    dbg_out = {}
    if stage < 9:
        allc = [("c", e_, sc.cnt[e_]) for e_ in ("pe", "act", "dve", "pool") if sc.cnt[e_] > 0]
        alld = [("d", k_, v_) for k_, v_ in sc.dcnt.items()]
        items = {"qTa": (qTa, [128, S], BF16), "kTa": (kTa, [128, S], BF16), "qTb": (qTb, [128, S], BF16),
                 "kTb": (kTb, [128, S], BF16), "Vab": (Vab, [128, NT * 256], BF16), "flog": (flog, [128, NT], F32),
                 "kmT": (kmT, [128, 32], F32), "rstd": (rstd, [128, NT], F32)}
        if stage >= 2:
            items["ctm"] = (ctm, [128, NT], F32)
            items["maskTM"] = (maskTM, [128, NT * 32], BF16)
        if stage >= 5:
            items = {"mT": (mT, [128, KC * TOK], BF16), "attnT": (attnT, [128, 16 * TOK], BF16)}
        if stage >= 6:
            items = {"xres": (xres, [128, 8 * D], F32)}
        if stage >= 7:
            items = {"xres": (xres, [128, 8 * D], F32)}
        if stage == 4:
            items = {"attnT": (attnT, [128, 16 * TOK], BF16)}
            alld = [("d", k_, v_) for k_, v_ in sc.dcnt.items()]
        for nm, (t_, shp, dt_) in items.items():
            dd = nc.dram_tensor("dbg_" + nm, shp, dt_, kind="ExternalOutput")
            dbg_out[nm] = (shp, dt_)
            flat = t_[:, :] if len(t_.shape) == 2 else t_[:, :, :].rearrange("p a b -> p (a b)")
            hdl = sc.dma("sp", lambda e, dd=dd, flat=flat: e.dma_start(out=dd.ap(), in_=flat), "dbg",
                         deps=allc + alld)
            final_waits.append(hdl)
        if stage >= 2 and stage < 5:
            agin_d = agin_l[0]
            dd = nc.dram_tensor("dbg_agin", [256, SPLITS[0][1]], BF16, kind="ExternalOutput")
            dbg_out["agin"] = ([256, SPLITS[0][1]], BF16)
            for r_ in range(2):
                hdl = sc.dma("sp", lambda e, dd=dd, r_=r_: e.dma_start(
                    out=dd.ap()[r_ * 128:(r_ + 1) * 128, :], in_=agin_d.ap()[r_ * 128:(r_ + 1) * 128, :]), "dbg",
                    deps=allc + alld + ob_handles)
                final_waits.append(hdl)
    if not final_waits:
        final_waits = [("d", k_, v_) for k_, v_ in sc.dcnt.items()]
    sc.wait("sp", final_waits)

    from contextlib import ExitStack
    with ExitStack() as es:
        csem = {e_: es.enter_context(nc.semaphore("s_" + e_)) for e_ in ("pe", "act", "dve", "pool")}
        dsem = {k_: es.enter_context(nc.semaphore("d_" + k_)) for k_ in sc.dcnt}
        block = es.enter_context(nc.Block())

        def run(engobj, en):
            waited = {}
            for fn, deps, signal, ds_, inc in sc.streams[en]:
                for d in deps:
                    key = (d[0], d[1])
                    if waited.get(key, 0) >= d[2]:
                        continue
                    sem = csem[d[1]] if d[0] == "c" else dsem[d[1]]
                    engobj.wait_ge(sem, d[2])
                    waited[key] = d[2]
                if fn is None:
                    continue
                ins = fn(engobj)
                if signal:
                    ins.then_inc(csem[en], 1)
                if ds_ is not None:
                    ins.then_inc(dsem[ds_], inc)

        @block.tensor
        def _(e):
            run(e, "pe")

        @block.scalar
        def _(e):
            run(e, "act")

        @block.vector
        def _(e):
            run(e, "dve")

        @block.gpsimd
        def _(e):
            run(e, "pool")

        @block.sync
        def _(e):
            run(e, "sp")
    return nc, dbg_out


def _consts(c):
    import ml_dtypes
    slope = float(2.0 ** (-(c + 1)))
    p = np.arange(128)
    identf = np.eye(128, dtype=np.float32)
    e = np.zeros((36, 32, 128), dtype=np.float32)
    for n in range(32):
        e[n, n, :] = 1.0
    e[32:36] = 1.0
    chunk = np.arange(512) // 128
    oh = (chunk[None, :] == np.arange(4)[:, None]).astype(np.float32)
    qend = (512.0 * np.arange(NQT)[:, None] + 128.0 * np.arange(4)[None, :] + 127.0)
    rowm = (-slope * qend / SCALE)[:, :, None] * oh[None, :, :]
    tri = np.where(p[:, None] <= p[None, :], 0.0, -BIG).astype(np.float32)
    kps = (slope * (128.0 * np.arange(NT)[None, :] + p[:, None])).astype(np.float32)
    qoff = np.broadcast_to((-slope * 512.0 * np.arange(NQT))[None, :], (128, NQT)).astype(np.float32)
    aql = (-slope * np.arange(512, dtype=np.float32))[None, :].astype(np.float32)
    return {
        "c_identb": identf.astype(ml_dtypes.bfloat16),
        "c_identf": identf,
        "c_triu": (p[:, None] <= p[None, :]).astype(np.float32),
        "c_su": (np.arange(64)[:, None] < np.arange(64)[None, :]).astype(np.float32),
        "c_tri": tri,
        "c_e": e.reshape(36, 32 * 128).astype(ml_dtypes.bfloat16),
        "c_rowm": rowm.reshape(NQT * 4, 512).astype(ml_dtypes.bfloat16),
        "c_oh": oh,
        "c_trib": tri.astype(ml_dtypes.bfloat16),
        "c_kps": kps,
        "c_qoff": np.ascontiguousarray(qoff),
        "c_aql": aql,
    }


_CACHE = {}


def make_in_maps(x, norm_mix_g, w_in, b_forget, w_proj_a, w_proj_b, w_out, norm_mlp_g, w_up, w_down,
                 norm_final_g):
    f = np.float32
    x2 = np.ascontiguousarray(np.asarray(x, dtype=f).reshape(S, D))
    w_in = np.asarray(w_in, dtype=f)
    w_gate = np.ascontiguousarray(w_in[:, 6152:6152 + 2 * D])
    shared = {
        "x": x2,
        "w_gate": w_gate,
        "w_proj_a": np.ascontiguousarray(np.asarray(w_proj_a, dtype=f)),
        "w_proj_b": np.ascontiguousarray(np.asarray(w_proj_b, dtype=f)),
        "w_out": np.ascontiguousarray(np.asarray(w_out, dtype=f)),
        "w_up": np.ascontiguousarray(np.asarray(w_up, dtype=f)),
        "w_down": np.ascontiguousarray(np.asarray(w_down, dtype=f)),
        "g_mix": np.asarray(norm_mix_g, dtype=f).reshape(1, D),
        "g_mlp": np.asarray(norm_mlp_g, dtype=f).reshape(1, D),
        "g_final": np.asarray(norm_final_g, dtype=f).reshape(1, D),
    }
    in_maps = []
    for c in range(NCORES):
        cols = []
        for base_ in (0, 1024, 3072, 4096, 2048, 5120):
            cols.append(w_in[:, base_ + c * 128: base_ + (c + 1) * 128])
        cols.append(w_in[:, 6144 + c: 6144 + c + 1])
        m = dict(shared)
        m["w_qkv"] = np.ascontiguousarray(np.concatenate(cols, axis=1))
        m["x_own"] = np.ascontiguousarray(x2[own_rows(c)])
        m["b_for"] = np.asarray(b_forget, dtype=f)[c].reshape(1, 1)
        m.update(_consts(c))
        in_maps.append(m)
    return in_maps


def kernel(x, norm_mix_g, w_in, b_forget, w_proj_a, w_proj_b, w_out, norm_mlp_g, w_up, w_down, norm_final_g):
    if "nc" not in _CACHE:
        _CACHE["nc"] = build_nc(9)[0]
    nc = _CACHE["nc"]
    in_maps = make_in_maps(x, norm_mix_g, w_in, b_forget, w_proj_a, w_proj_b, w_out, norm_mlp_g, w_up,
                           w_down, norm_final_g)
    res = run_bass_kernel_spmd(nc, in_maps, core_ids=list(range(NCORES)))
    ys = [np.asarray(res.results[c]["y"], dtype=np.float32) for c in range(NCORES)]
    out = np.empty((S, D), dtype=np.float32)
    for c in range(NCORES):
        out[own_rows(c)] = ys[c]
    return out.reshape(1, S, D)
```

```python
import os
import numpy as np
import concourse.bass as bass
import concourse.mybir as mybir
from concourse.bass_utils import run_bass_kernel_spmd

F32 = mybir.dt.float32
BF16 = mybir.dt.bfloat16
U8 = mybir.dt.uint8
AF = mybir.ActivationFunctionType
ALU = mybir.AluOpType
AX = mybir.AxisListType

NCORES = 8
S = 8192
D = 2048
DH = 128
KC = D // 128
TOK = S // NCORES
DFF = 4 * D
EPS = 1e-6
SCALE = DH ** -0.5
BIG = 30000.0
NT = S // 128
NQT = S // 512
WQ = 6 * 128 + 1
SPLITS = [(0, 5120), (5120, 7168), (7168, 7680), (7680, 8192)]


def own_rows(c):
    rows = []
    for a, b in SPLITS:
        w = (b - a) // NCORES
        rows.append(np.arange(a + c * w, a + (c + 1) * w))
    return np.concatenate(rows)

STAGE = int(os.environ.get("MK_STAGE", "9"))


class Sched:
    ENGS = ("pe", "act", "dve", "pool", "sp")

    def __init__(self):
        self.streams = {e: [] for e in self.ENGS}
        self.cnt = {e: 0 for e in self.ENGS}
        self.dcnt = {}

    def op(self, eng, fn, deps=(), signal=True):
        deps = [d for d in deps if d is not None]
        h = None
        if signal:
            self.cnt[eng] += 1
            h = ("c", eng, self.cnt[eng])
        self.streams[eng].append((fn, deps, signal, None, 0))
        return h

    def dma(self, eng, fn, sem, deps=(), inc=16):
        deps = [d for d in deps if d is not None]
        self.dcnt[sem] = self.dcnt.get(sem, 0) + inc
        h = ("d", sem, self.dcnt[sem])
        self.streams[eng].append((fn, deps, False, sem, inc))
        return h

    def wait(self, eng, deps):
        deps = [d for d in deps if d is not None]
        self.streams[eng].append((None, deps, False, None, 0))


def build_nc(stage=STAGE):
    nc = bass.Bass("TRN2", target_bir_lowering=False)
    sc = Sched()

    def din(name, shape, dt=F32):
        return nc.dram_tensor(name, list(shape), dt, kind="ExternalInput")

    x_d = din("x", [S, D])
    xo_d = din("x_own", [TOK, D])
    wqkv_d = din("w_qkv", [D, WQ])
    wg_d = din("w_gate", [D, 2 * D])
    wpa_d = din("w_proj_a", [1024, D])
    wpb_d = din("w_proj_b", [1024, D])
    wo_d = din("w_out", [D, D])
    wu_d = din("w_up", [D, DFF])
    wd_d = din("w_down", [DFF, D])
    g1_d = din("g_mix", [1, D])
    g2_d = din("g_mlp", [1, D])
    g3_d = din("g_final", [1, D])
    bf_d = din("b_for", [1, 1])
    cidb_d = din("c_identb", [128, 128], BF16)
    cidf_d = din("c_identf", [128, 128])
    ctriu_d = din("c_triu", [128, 128])
    csu_d = din("c_su", [64, 64])
    ctri_d = din("c_tri", [128, 128])
    ce_d = din("c_e", [36, 32 * 128], BF16)
    crowm_d = din("c_rowm", [NQT * 4, 512], BF16)
    coh_d = din("c_oh", [4, 512])
    ctrib_d = din("c_trib", [128, 128], BF16)
    ckps_d = din("c_kps", [128, NT])
    cqoff_d = din("c_qoff", [128, NQT])
    caql_d = din("c_aql", [1, 512])
    y_d = nc.dram_tensor("y", [TOK, D], F32, kind="ExternalOutput")
    agin_l = [nc.dram_tensor(f"ag_in{i}", [256, SPLITS[i][1] - SPLITS[i][0]], BF16, kind="Internal")
              for i in range(len(SPLITS))]
    agout_l = [nc.dram_tensor(f"ag_out{i}", [NCORES * 256, SPLITS[i][1] - SPLITS[i][0]], BF16, kind="Internal")
               for i in range(len(SPLITS))]
    cdram_d = nc.dram_tensor("c_scr", [64, 128], F32, kind="Internal")
    dbg = {}
    if stage < 9:
        dbg_d = {}

    def bcast(handle, off, n):
        return bass.AP(handle, off, [[0, 128], [1, n]])

    arena_g = nc.sbuf_tensor("arena", [128, 212000], U8)
    arena_g.__enter__()
    base = nc.lookup_mloc("arena").addr
    cur = [0]

    def salloc(name, shape, dt, at=None):
        esz = 4 if dt == F32 else 2
        n = 1
        for s_ in shape[1:]:
            n *= s_
        nbytes = (n * esz + 31) // 32 * 32
        if at is None:
            off = cur[0]
            cur[0] += nbytes
        else:
            off = at
        assert off + nbytes <= 212000, (name, off, nbytes)
        return nc.alloc_sbuf_tensor_at(name, list(shape), dt, offset=base + off), off, nbytes

    offs = {}

    def A(name, shape, dt, at=None):
        t_, off_, _ = salloc(name, shape, dt, at)
        offs[name] = off_
        return t_

    identb = A("identb", [128, 128], BF16)
    identf = A("identf", [128, 128], F32)
    onesb = A("onesb", [128, 128], BF16)
    onesf = A("onesf", [128, 128], F32)
    triu = A("triu", [128, 128], F32)
    su64 = A("su64", [64, 64], F32)
    tri = A("tri", [128, 128], F32)
    gB = A("gB", [128, D], F32)
    ssq = A("ssq", [128, NT], F32)
    rstd = A("rstd", [128, NT], F32)
    flog = A("flog", [128, NT], F32)
    kmT = A("kmT", [128, 32], F32)
    kps = A("kps", [128, NT], F32)
    qoff = A("qoff", [128, NQT], F32)
    bfor = A("bfor", [128, 1], F32)
    ctm = A("ctm", [128, NT], F32)
    negc = A("negc", [128, NT], F32)
    sm1 = A("sm1", [128, NT], F32)
    sm2 = A("sm2", [128, NT], F32)
    sm3 = A("sm3", [128, NT], F32)
    cT = A("cT", [64, 128], F32)
    bm64 = A("bm64", [64, 64], F32)
    tot64 = A("tot64", [64, 1], F32)
    gsb = [A(f"gsb{i}", [128, 32], F32) for i in range(2)]
    mx8 = [A(f"mx8{i}", [128, 8], F32) for i in range(2)]
    selt = [A(f"selt{i}", [128, 32], F32) for i in range(2)]
    biasqt = [A(f"biasqt{i}", [128, NT], F32) for i in range(2)]
    ssq2 = A("ssq2", [128, 8], F32)
    rstd2 = A("rstd2", [128, 8], F32)
    maskTM = A("maskTM", [128, NT, 32], BF16)
    P_CONST_END = cur[0]

    qTa = A("qTa", [128, S], BF16)
    kTa = A("kTa", [128, S], BF16)
    qTb = A("qTb", [128, S], BF16)
    kTb = A("kTb", [128, S], BF16)
    Vab = A("Vab", [128, NT, 256], BF16)
    P12_END = cur[0]
    wqkv = A("wqkv", [128, KC, WQ], BF16)
    xs = [A(f"xs{i}", [128, D], F32) for i in range(3)]
    xb = [A(f"xb{i}", [128, D], BF16) for i in range(2)]
    hnT = [A(f"hnT{i}", [128, KC, 512], BF16) for i in range(2)]
    qf32 = A("qf32", [128, 512], F32)
    P1_END = cur[0]
    cur[0] = P12_END
    Esel = A("Esel", [128, 32 * 128], BF16)
    aql = A("aql", [128, 512], F32)
    tbuf = [A(f"tbuf{i}", [128, 512], F32) for i in range(4)]
    pT = [A(f"pT{i}", [128, 512], BF16) for i in range(4)]
    cqB = [A(f"cqB{i}", [128, 512], F32) for i in range(2)]
    obuf = [A(f"obuf{i}", [128, 512], BF16) for i in range(4)]
    rden = [A(f"rden{i}", [128, 512], F32) for i in range(2)]
    maskTq = [A(f"maskTq{i}", [128, 512], BF16) for i in range(2)]
    pT += [A(f"pT{i}", [128, 512], BF16) for i in range(4, 8)]
    dsum = [A(f"dsum{i}", [128, 512], F32) for i in range(2)]
    ones32 = A("ones32", [128, 128], F32)
    trib = A("trib", [128, 128], BF16)
    oh4 = A("oh4", [128, 512], F32)
    crefs = A("crefs", [128, NQT], F32)
    Rf = [A(f"Rf{i}", [128, 512], BF16) for i in range(2)]
    ones4 = A("ones4", [128, 128], BF16)
    P2_END = cur[0]
    cur[0] = P_CONST_END
    regAB = cur[0]
    hT = A("hT", [128, KC, TOK], BF16)
    attnT = A("attnT", [128, 16, TOK], BF16)
    xres = A("xres", [128, 8, D], F32, at=regAB)
    mT = A("mT", [128, KC, TOK], BF16)
    wring = [A(f"wring{i}", [128, 8192], BF16) for i in range(3)]
    actT = [A(f"actT{i}", [128, 4, TOK], BF16) for i in range(2)]
    tmp4 = [A(f"tmp4{i}", [128, 512], F32) for i in range(4)]
    xs4 = A("xs4", [128, D], F32)
    xb4 = A("xb4", [128, D], BF16)
    P4_END = cur[0]
    assert max(P1_END, P2_END, P4_END) <= 212000, (P1_END, P2_END, P4_END)

    banks = []
    for b in range(8):
        g_ = nc.psum_tensor(f"bank{b}", [128, 512], F32)
        banks.append(g_.__enter__())

    def bank_bf(b):
        return banks[b].bitcast(BF16) if hasattr(banks[b], "bitcast") else None

    bank_last = [None] * 8

    cl = []
    PRELOAD = []
    cl.append(sc.dma("sp", lambda e: e.dma_start(out=identb[:, :], in_=cidb_d.ap()), "const"))
    cl.append(sc.dma("sp", lambda e: e.dma_start(out=identf[:, :], in_=cidf_d.ap()), "const"))
    cl.append(sc.dma("sp", lambda e: e.dma_start(out=triu[:, :], in_=ctriu_d.ap()), "const"))
    cl.append(sc.dma("sp", lambda e: e.dma_start(out=su64[:, :], in_=csu_d.ap()), "const"))
    cl.append(sc.dma("sp", lambda e: e.dma_start(out=tri[:, :], in_=ctri_d.ap()), "const"))
    cl.append(sc.dma("sp", lambda e: e.dma_start(out=kps[:, :], in_=ckps_d.ap()), "const"))
    cl.append(sc.dma("sp", lambda e: e.dma_start(out=qoff[:, :], in_=cqoff_d.ap()), "const"))
    cl.append(sc.dma("sp", lambda e: e.dma_start(out=bfor[:, :], in_=bcast(bf_d, 0, 1)), "const"))
    cl.append(sc.dma("sp", lambda e: e.dma_start(out=gB[:, :], in_=bcast(g1_d, 0, D)), "const"))
    CONST = cl[-1]
    sc.op("pool", lambda e: e.memset(onesb[:, :], 1.0))
    sc.op("pool", lambda e: e.memset(onesf[:, :], 1.0))
    sc.op("pool", lambda e: e.memset(ssq[:, :], 0.0))
    sc.op("pool", lambda e: e.memset(ssq2[:, :], 0.0))
    POOLINIT = sc.op("pool", lambda e: e.memset(kmT[:, :], 0.0))
    wq_src = wqkv_d.ap().rearrange("(k p) c -> p k c", p=128)
    wq_hs = []
    for s_ in range(5):
        c0_, c1_ = (s_ * 128, (s_ + 1) * 128) if s_ < 4 else (512, WQ)
        wq_hs.append(sc.dma("pool", lambda e, c0_=c0_, c1_=c1_: e.dma_start(
            out=wqkv[:, :, c0_:c1_], in_=wq_src[:, :, c0_:c1_]), f"wq{s_}"))

    st = {"sq": {}, "xb": {}, "tr": {}, "ev": {}}

    def norm_tile(key, src_ap, xs_t, xb_t, ssq_col, rstd_col, extra_deps=(), load_deps=(), in_sbuf=None):
        if in_sbuf is None:
            ld = sc.dma("sp", lambda e: e.dma_start(out=xs_t[:, :], in_=src_ap), "xs_" + key[0] + str(key[2]),
                        deps=list(load_deps))
            xin = xs_t[:, :]
        else:
            ld = None
            xin = in_sbuf
        sq = sc.op("act", lambda e: e.activation(out=xb_t[:, :], in_=xin, func=AF.Square,
                                                 accum_out=ssq_col),
                   deps=[ld, POOLINIT] + list(extra_deps))
        r0 = sc.op("dve", lambda e: e.tensor_scalar(out=rstd_col, in0=ssq_col, scalar1=1.0 / D,
                                                    scalar2=EPS, op0=ALU.mult, op1=ALU.add), deps=[sq])
        r1 = sc.op("act", lambda e: e.activation(out=rstd_col, in_=rstd_col, func=AF.Sqrt), deps=[r0])
        r2 = sc.op("dve", lambda e: e.reciprocal(out=rstd_col, in_=rstd_col), deps=[r1])
        return sq, r2, xin

    tp_banks = [(0, 1), (2, 3)]
    pj_banks = [4, 5, 6]
    pj_i = [0]
    GPB = 7
    hn_readers = [None, None]
    xs_free = [None, None, None]
    xb_free = [None, None]
    ev_last = {}
    gate_state = {}

    p1_ld = {}

    def p1_load(i):
        s3 = i % 3
        p1_ld[i] = sc.dma("sp", lambda e: e.dma_start(out=xs[s3][:, :], in_=x_d.ap()[i * 128:(i + 1) * 128, :]),
                          f"xs_a{s3}", deps=[xs_free[s3]])

    def p1_A(i):
        sl = i % 2
        s3 = i % 3
        if i + 2 < NT:
            p1_load(i + 2)
        sq, r2, xin = norm_tile(("a", i, sl), None, None, xb[sl],
                                ssq[:, i:i + 1], rstd[:, i:i + 1],
                                extra_deps=[xb_free[sl], p1_ld[i]], in_sbuf=xs[s3][:, :])
        h = sc.op("dve", lambda e: e.scalar_tensor_tensor(out=xb[sl][:, :], in0=xs[s3][:, :],
                                                          scalar=rstd[:, i:i + 1], in1=gB[:, :],
                                                          op0=ALU.mult, op1=ALU.mult),
                  deps=[r2, CONST, sq])
        xs_free[s3] = h
        st["xb"][i] = h

    def transposes(src_xb, bankpair, dep, dst_fn, evdeps):
        hs = []
        for half in range(2):
            b = bankpair[half]
            pb = banks[b].bitcast(BF16)
            last = None
            for kk in range(8):
                k = half * 8 + kk
                last = sc.op("pe", lambda e, pb=pb, kk=kk, k=k: e.transpose(
                    out=pb[:, kk * 128:(kk + 1) * 128], in_=src_xb[:, k * 128:(k + 1) * 128],
                    identity=identb[:, :]),
                    deps=[dep, CONST, bank_last[b]], signal=(kk == 7))
            src = pb[:, :].rearrange("p (k t) -> p k t", t=128)
            if half == 0:
                ev = sc.op("act", lambda e, src=src: e.activation(out=dst_fn(0), in_=src, func=AF.Copy),
                           deps=[last] + list(evdeps))
            else:
                ev = sc.op("dve", lambda e, src=src: e.tensor_copy(out=dst_fn(1), in_=src),
                           deps=[last] + list(evdeps))
            bank_last[b] = ev
            hs.append((last, ev))
        return hs

    def p1_B(i):
        sl = i % 2
        g, sub = i // 4, i % 4
        hs = transposes(xb[sl], tp_banks[sl], st["xb"][i],
                        lambda half: hnT[g % 2][:, half * 8:(half + 1) * 8, sub * 128:(sub + 1) * 128],
                        [hn_readers[g % 2]])
        xb_free[sl] = hs[1][0]
        ev_last[i] = [hs[0][1], hs[1][1]]

    def next_pj():
        b = pj_banks[pj_i[0] % 3]
        pj_i[0] += 1
        return b

    def p1_C(g, s):
        hsl = hnT[g % 2]
        evd = ev_last[4 * g + 3] + ev_last[4 * g + 2] + ev_last[4 * g + 1] + ev_last[4 * g]
        b = next_pj()
        last = None
        for k in range(KC):
            last = sc.op("pe", lambda e, b=b, k=k: e.matmul(
                banks[b][:, :], lhsT=wqkv[:, k, s * 128:(s + 1) * 128], rhs=hsl[:, k, :],
                start=(k == 0), stop=(k == KC - 1)),
                deps=evd + [wq_hs[s], bank_last[b]], signal=(k == KC - 1))
        cols = slice(g * 512, (g + 1) * 512)
        if s == 0:
            e1 = sc.op("act", lambda e, b=b: e.activation(out=qf32[:, :], in_=banks[b][:, :], func=AF.Copy),
                       deps=[last, gate_state.get("qf_free"), gate_state.get("qf_rd")])
            e2 = sc.op("dve", lambda e: e.tensor_copy(out=qTa[:, cols], in_=qf32[:, :]), deps=[e1])
            bank_last[b] = e1
            gate_state["qf"] = e1
            gate_state["qf_rd"] = e2
        elif s == 1:
            ee = None
            for h2 in range(2):
                ee = sc.op("act", lambda e, b=b, h2=h2: e.activation(
                    out=kTa[:, g * 512 + h2 * 256: g * 512 + (h2 + 1) * 256],
                    in_=banks[b][:, h2 * 256:(h2 + 1) * 256], func=AF.Copy,
                    accum_out=kmT[:, 2 * g + h2: 2 * g + h2 + 1]), deps=[last, POOLINIT])
            bank_last[b] = ee
            gate_state["km"] = ee
        elif s == 2:
            ee = sc.op("dve", lambda e, b=b: e.tensor_copy(out=qTb[:, cols], in_=banks[b][:, :]), deps=[last])
            bank_last[b] = ee
        else:
            ee = sc.op("act", lambda e, b=b: e.activation(out=kTb[:, cols], in_=banks[b][:, :], func=AF.Copy),
                       deps=[last])
            bank_last[b] = ee
        b = next_pj()
        i = 4 * g + s
        last = None
        for k in range(KC):
            last = sc.op("pe", lambda e, b=b, k=k: e.matmul(
                banks[b][:, 0:257], lhsT=hsl[:, k, s * 128:(s + 1) * 128], rhs=wqkv[:, k, 512:769],
                start=(k == 0), stop=(k == KC - 1)),
                deps=evd + [wq_hs[4], bank_last[b]], signal=(k == KC - 1))
        if s == 3:
            hn_readers[g % 2] = last
        e1 = sc.op("dve", lambda e, b=b: e.tensor_copy(out=Vab[:, i, :], in_=banks[b][:, 0:256]), deps=[last])
        e2 = sc.op("act", lambda e, b=b: e.activation(out=flog[:, i:i + 1], in_=banks[b][:, 256:257],
                                                      func=AF.Copy), deps=[last, e1])
        bank_last[b] = e2
        if s == 1:
            glast = None
            for u in range(4):
                ti = 4 * g + u
                blk = ti // 2
                gs = gsb[u % 2]
                m8 = mx8[u % 2]
                se = selt[u % 2]
                d0 = sc.op("pool", lambda e, gs=gs: e.memset(gs[:, :], -BIG), deps=[gate_state.get(("gsrd", u % 2))])
                if blk > 0:
                    gm = sc.op("pe", lambda e, u=u, blk=blk: e.matmul(
                        banks[GPB][:, u * 32:u * 32 + blk], lhsT=qf32[:, u * 128:(u + 1) * 128],
                        rhs=kmT[:, 0:blk], start=True, stop=True),
                        deps=[gate_state["qf"], gate_state["km"], bank_last[GPB]])
                    glast = gm
                    d0 = sc.op("dve", lambda e, gs=gs, u=u, blk=blk: e.tensor_copy(
                        out=gs[:, 0:blk], in_=banks[GPB][:, u * 32:u * 32 + blk]), deps=[gm, d0])
                    gate_state["gp_rd"] = d0
                    bank_last[GPB] = d0
                d1 = sc.op("dve", lambda e, gs=gs, m8=m8: e.max(out=m8[:, :], in_=gs[:, :]), deps=[d0])
                d2 = sc.op("dve", lambda e, m8=m8: e.tensor_scalar_max(out=m8[:, 2:3], in0=m8[:, 2:3],
                                                                        scalar1=-BIG / 2), deps=[d1])
                d3 = sc.op("dve", lambda e, gs=gs, m8=m8, se=se: e.tensor_scalar(
                    out=se[:, :], in0=gs[:, :], scalar1=m8[:, 2:3], scalar2=None, op0=ALU.is_ge), deps=[d2])
                d4 = sc.op("dve", lambda e, se=se, blk=blk: e.memset(se[:, blk:blk + 1], 1.0), deps=[d3])
                d5 = sc.op("dve", lambda e, se=se, ti=ti: e.tensor_scalar(
                    out=maskTM[:, ti, :], in0=se[:, :], scalar1=1.0, scalar2=BIG,
                    op0=ALU.subtract, op1=ALU.mult), deps=[d4])
                gate_state[("gsrd", u % 2)] = d5
            if glast is not None:
                gate_state["qf_free"] = glast

    NT1 = NT if stage >= 1 else 0
    if NT1:
        n_sp0 = len(sc.streams["sp"])
        p1_load(0)
        p1_load(1)
        first2 = sc.streams["sp"][n_sp0:n_sp0 + 2]
        del sc.streams["sp"][n_sp0:n_sp0 + 2]
        sc.streams["sp"][0:0] = first2
        p1_A(0)
        for i in range(NT1):
            if i + 1 < NT1:
                p1_A(i + 1)
            p1_B(i)
            if i >= 4:
                p1_C(i // 4 - 1, i % 4)
        for s in range(4):
            p1_C(NT1 // 4 - 1, s)

    final_waits = []
    ob_handles = []
    if stage >= 2:
        P1DONE_PE = ("c", "pe", sc.cnt["pe"])
        P1DONE_ACT = ("c", "act", sc.cnt["act"])
        P1DONE_DVE = ("c", "dve", sc.cnt["dve"])
        p1done = [P1DONE_PE, P1DONE_ACT, P1DONE_DVE]
        sc.op("pool", lambda e: e.memset(ones32[:, :], 1.0 / 32.0), deps=p1done)
        sc.op("pool", lambda e: e.memset(ones4[:, :], 0.0), deps=p1done)
        sc.op("pool", lambda e: e.memset(Rf[0][:, :], 0.0), deps=p1done)
        sc.op("pool", lambda e: e.memset(Rf[1][:, :], 0.0), deps=p1done)
        sc.op("pool", lambda e: e.memset(Esel[:, :], 0.0), deps=p1done)
        sc.op("pool", lambda e: e.memset(maskTq[0][:, :], 0.0), deps=p1done)
        sc.op("pool", lambda e: e.memset(maskTq[1][:, :], 0.0), deps=p1done)
        PAD0 = ("c", "pool", sc.cnt["pool"])
        c2 = sc.dma("sp", lambda e: e.dma_start(out=Esel[0:36, :], in_=ce_d.ap()), "const2", deps=p1done + [PAD0])
        c2 = sc.dma("sp", lambda e: e.dma_start(out=ones4[32:36, :], in_=ce_d.ap()[32:36, 0:128]), "const2",
                    deps=p1done + [PAD0])
        c2 = sc.dma("sp", lambda e: e.dma_start(out=trib[:, :], in_=ctrib_d.ap()), "const2", deps=p1done)
        c2 = sc.dma("sp", lambda e: e.dma_start(out=oh4[32:36, :], in_=coh_d.ap()), "const2", deps=p1done)
        c2 = sc.dma("sp", lambda e: e.dma_start(out=aql[:, :], in_=bcast(caql_d, 0, 512)), "const2", deps=p1done)
        CONST2 = c2
        for b in range(8):
            bank_last[b] = None

        z = sc.op("dve", lambda e: e.tensor_scalar(out=sm1[:, :], in0=flog[:, :], scalar1=bfor[:, 0:1],
                                                   scalar2=None, op0=ALU.add), deps=p1done + [CONST])
        az = sc.op("act", lambda e: e.activation(out=sm2[:, :], in_=sm1[:, :], func=AF.Abs), deps=[z] + p1done)
        ex = sc.op("act", lambda e: e.activation(out=sm2[:, :], in_=sm2[:, :], func=AF.Exp, scale=-1.0),
                   deps=[az] + p1done)
        ln = sc.op("act", lambda e: e.activation(out=sm2[:, :], in_=sm2[:, :], func=AF.Ln, bias=1.0),
                   deps=[ex])
        mn = sc.op("dve", lambda e: e.tensor_scalar_min(out=sm1[:, :], in0=sm1[:, :], scalar1=0.0), deps=[z, ln])
        lf = sc.op("dve", lambda e: e.tensor_sub(out=sm3[:, :], in0=sm1[:, :], in1=sm2[:, :]), deps=[mn, ln])
        t1 = sc.op("pe", lambda e: e.transpose(out=banks[0][0:64, 0:128], in_=sm3[:, :], identity=identf[:, :]),
                   deps=[lf] + p1done)
        tt_ = sc.op("dve", lambda e: e.reduce_sum(out=tot64[:, :], in_=banks[0][0:64, 0:128], axis=AX.X), deps=[t1])
        bmh = sc.op("dve", lambda e: e.tensor_scalar(out=bm64[:, :], in0=su64[:, :], scalar1=tot64[:, 0:1],
                                                     scalar2=None, op0=ALU.mult), deps=[tt_])
        sc.op("pe", lambda e: e.matmul(banks[1][:, 0:64], lhsT=triu[:, :], rhs=sm3[:, :], start=True, stop=False),
              deps=[lf], signal=False)
        cm = sc.op("pe", lambda e: e.matmul(banks[1][:, 0:64], lhsT=onesf[0:64, :], rhs=bm64[:, :],
                                            start=False, stop=True), deps=[bmh, POOLINIT])
        cc = sc.op("dve", lambda e: e.tensor_copy(out=ctm[:, :], in_=banks[1][:, 0:64]), deps=[cm])
        ng = sc.op("dve", lambda e: e.tensor_scalar_mul(out=negc[:, :], in0=ctm[:, :], scalar1=-1.0), deps=[cc])
        t2 = sc.op("pe", lambda e: e.transpose(out=banks[0][0:64, 0:128], in_=ctm[:, :], identity=identf[:, :]),
                   deps=[cc, tt_])
        ct_h = sc.op("dve", lambda e: e.tensor_copy(out=cT[:, :], in_=banks[0][0:64, 0:128]), deps=[t2])
        cst = sc.dma("sp", lambda e: e.dma_start(out=cdram_d.ap(), in_=cT[:, :]), "cst", deps=[ct_h])
        crf = sc.dma("sp", lambda e: e.dma_start(out=crefs[32:36, :],
                                                 in_=bass.AP(cdram_d, 127, [[128, 4], [512, NQT]]),
                                                 allow_slow_non_contiguous=True),
                     "crf", deps=[cst])
        bank_last[0] = ct_h
        bank_last[1] = ng
        dbg["ctm"] = (ctm, [128, NT], F32, [ng])

        SBK = [0, 1, 2, 3]
        OB = {0: 4, 1: 6}
        DB = {0: 5, 1: 7}
        sb_i = [0]
        t_rd = [None] * 4
        p_rd = [None] * 8
        pend = {0: [], 1: []}
        dsum_free = [None, None]
        cq_rd = [None, None]
        mq_rd = [None, None]
        bq_rd = [None, None]
        ob_free = [None] * 4
        rd_free = [None, None]
        ring_i = [0]

        sb_owner = {b_: None for b_ in SBK}

        def take_sb():
            for t_ in range(4):
                b_ = SBK[(sb_i[0] + t_) % 4]
                if sb_owner[b_] is None:
                    sb_i[0] = (sb_i[0] + t_ + 1) % 4
                    sb_owner[b_] = 1
                    return b_
            raise AssertionError("no free S^T bank")

        def tiles_for(typ, QT):
            return [(typ, QT, j) for j in range(4 * QT + 4)]

        def qt_prologue(typ, QT):
            sl = QT % 2
            if typ == 0:
                h = sc.op("pool", lambda e: e.tensor_scalar(out=Rf[sl][32:36, :], in0=oh4[32:36, :],
                                                            scalar1=crefs[32:36, QT:QT + 1], scalar2=1.0 / SCALE,
                                                            op0=ALU.mult, op1=ALU.mult),
                          deps=[crf, CONST2, cq_rd[sl]])
                return {"rf": Rf[sl], "rfh": h, "bias": negc, "biash": ng}
            b = take_sb()
            sb_owner[b] = None
            pb = banks[b].bitcast(BF16)
            last = None
            for u in range(4):
                last = sc.op("pe", lambda e, u=u, pb=pb: e.transpose(
                    out=pb[0:32, u * 128:(u + 1) * 128], in_=maskTM[:, 4 * QT + u, :], identity=identb[:, :]),
                    deps=[bank_last[b]] + p1done, signal=(u == 3))
            mh = sc.op("act", lambda e, pb=pb: e.activation(out=maskTq[sl][0:32, :], in_=pb[0:32, 0:512], func=AF.Copy),
                       deps=[last, mq_rd[sl], PAD0])
            bank_last[b] = mh
            rh = sc.dma("sp", lambda e: e.dma_start(out=maskTq[sl][32:36, :], in_=crowm_d.ap()[4 * QT:4 * QT + 4, :]),
                        f"rowm{sl}", deps=[mq_rd[sl], PAD0])
            return {"bias": kps, "biash": CONST, "mq": maskTq[sl], "mqh": mh, "rmh": rh}

        def qk(tile, ctx):
            typ, QT, j = tile
            b = take_sb()
            jj = j - 4 * QT
            q0 = 128 * jj if jj > 0 else 0
            diag = jj >= 0
            kT_ = kTb if typ == 0 else kTa
            qT_ = qTb if typ == 0 else qTa
            sc.op("pe", lambda e: e.matmul(banks[b][:, q0:512], lhsT=kT_[:, j * 128:(j + 1) * 128],
                                           rhs=qT_[:, QT * 512 + q0:(QT + 1) * 512],
                                           start=True, stop=False),
                  deps=[bank_last[b]] + p1done, signal=False)
            if typ == 0:
                h = sc.op("pe", lambda e: e.matmul(banks[b][:, q0:512], lhsT=ones4[:, :],
                                                   rhs=ctx["rf"][:, q0:512], start=False, stop=not diag),
                          deps=[ctx["rfh"], CONST2, PAD0], signal=not diag)
            else:
                n = j // 2
                h = sc.op("pe", lambda e: e.matmul(banks[b][:, q0:512], lhsT=Esel[:, n * 128:(n + 1) * 128],
                                                   rhs=ctx["mq"][:, q0:512], start=False, stop=not diag),
                          deps=[ctx["mqh"], ctx["rmh"], CONST2, PAD0], signal=not diag)
            if diag:
                h = sc.op("pe", lambda e: e.matmul(banks[b][:, q0:q0 + 128], lhsT=identb[:, :], rhs=trib[:, :],
                                                   start=False, stop=True), deps=[CONST, CONST2])
            return {"b": b, "q0": q0, "jj": jj, "qk": h}

        def rest(tile, ctx, info, first, last_):
            typ, QT, j = tile
            b, q0, jj = info["b"], info["q0"], info["jj"]
            r = ring_i[0] % 4
            rp = ring_i[0] % 8
            ring_i[0] += 1
            pt = pT[rp]
            h2 = sc.op("act", lambda e: e.activation(out=pt[:, q0:512], in_=banks[b][:, q0:512], func=AF.Exp,
                                                     bias=ctx["bias"][:, j:j + 1], scale=SCALE),
                       deps=[info["qk"], ctx["biash"], p_rd[rp]])
            bank_last[b] = h2
            sb_owner[b] = None
            ctx["bias_rd"] = h2
            vsl = slice(128, 256) if typ == 0 else slice(0, 128)
            sc.op("pe", lambda e: e.matmul(banks[OB[typ]][:, q0:512], lhsT=Vab[:, j, vsl], rhs=pt[:, q0:512],
                                           start=first, stop=last_),
                  deps=[h2, bank_last[OB[typ]] if first else None], signal=False)
            if QT == 0:
                h3 = sc.op("pe", lambda e: e.matmul(banks[DB[typ]][:, q0:512], lhsT=onesb[:, :], rhs=pt[:, q0:512],
                                                    start=first, stop=last_),
                           deps=[h2, POOLINIT, bank_last[DB[typ]] if first else None])
                p_rd[rp] = h3
                return h3
            pend[typ].append((pt, q0, rp, h2, j))
            h3 = None
            if j % 4 == 3:
                for g_, (pt_, q0_, rp_, h2_, j_) in enumerate(pend[typ]):
                    h3 = sc.op("pe", lambda e, g_=g_, pt_=pt_, q0_=q0_, j_=j_: e.matmul(
                        banks[DB[typ]][32 * g_:32 * g_ + 32, q0_:512], lhsT=onesb[:, 32 * g_:32 * g_ + 32],
                        rhs=pt_[:, q0_:512], start=(j_ < 4), stop=(j_ >= 4 * QT),
                        tile_position=(0, 32 * g_), skip_group_check=True),
                        deps=[h2_, POOLINIT, bank_last[DB[typ]] if j_ < 4 else None], signal=(g_ == 3))
                for (_, _, rp_, _, _) in pend[typ]:
                    p_rd[rp_] = h3
                pend[typ] = []
            return h3

        oi = [0]
        seg_done = {}
        ag_h = []

        def qt_epilogue(typ, QT, ctx, lastpv):
            sl = typ
            o = oi[0] % 4
            oi[0] += 1
            if QT > 0:
                c1 = sc.op("act", lambda e: e.activation(out=dsum[sl][:, :], in_=banks[DB[typ]][:, :], func=AF.Copy),
                           deps=[lastpv, dsum_free[sl]])
                lastpv = sc.op("pe", lambda e: e.matmul(banks[DB[typ]][:, :], lhsT=ones32[:, :], rhs=dsum[sl][:, :],
                                                        start=True, stop=True), deps=[c1, PAD0])
                dsum_free[sl] = lastpv
            r1 = sc.op("dve", lambda e: e.reciprocal(out=rden[sl][:, :], in_=banks[DB[typ]][:, :]),
                       deps=[lastpv, rd_free[sl]])
            r2 = sc.op("dve", lambda e: e.tensor_tensor(out=obuf[o][:, :], in0=banks[OB[typ]][:, :],
                                                        in1=rden[sl][:, :], op=ALU.mult),
                       deps=[r1, ob_free[o]])
            rd_free[sl] = r2
            bank_last[DB[typ]] = r1
            bank_last[OB[typ]] = r2
            row0 = 0 if typ == 1 else 128
            si = [i for i, (a_, b_) in enumerate(SPLITS) if a_ <= QT * 512 < b_][0]
            c0 = QT * 512 - SPLITS[si][0]
            dh = sc.dma("sp", lambda e: e.dma_start(out=agin_l[si].ap()[row0:row0 + 128, c0:c0 + 512],
                                                    in_=obuf[o][:, :]), f"ob{o}", deps=[r2])
            ob_free[o] = dh
            ob_handles.append(dh)
            seg_done[(typ, QT)] = True
            if (QT + 1) * 512 == SPLITS[si][1] and seg_done.get((1 - typ, QT)):
                ag_h.append(sc.dma("pool", lambda e: e.collective_compute(
                    "AllGather", ALU.bypass, replica_groups=[list(range(NCORES))],
                    ins=[agin_l[si].ap()], outs=[agout_l[si].ap()]), "cc",
                    deps=[h_ for h_ in ob_free if h_ is not None], inc=1))
            if typ == 0:
                cq_rd[QT % 2] = ctx["last_qk"]
            else:
                mq_rd[QT % 2] = ctx["last_qk"]
                bq_rd[QT % 2] = ctx["bias_rd"]

        seq = []
        nqt = NQT if stage >= 3 else 2
        for QT in range(nqt):
            for typ in (0, 1):
                tl = tiles_for(typ, QT)
                for idx, t in enumerate(tl):
                    seq.append((t, idx == 0, idx == len(tl) - 1))
        LOOK = 3
        HOIST = 6
        ctxs = {}
        infos = {}

        def maybe_prologue(k, force):
            if k >= len(seq):
                return
            tile, first, _ = seq[k]
            key = (tile[0], tile[1])
            if not first or key in ctxs:
                return
            if force or tile[1] == 0 or seg_done.get((tile[0], tile[1] - 1)):
                ctxs[key] = qt_prologue(tile[0], tile[1])

        for n in range(len(seq) + LOOK):
            if n < len(seq):
                for k_ in range(n + 1, n + HOIST + 1):
                    maybe_prologue(k_, False)
                maybe_prologue(n, True)
                tile, first, last_ = seq[n]
                ctx = ctxs[(tile[0], tile[1])]
                infos[n] = qk(tile, ctx)
                ctx["last_qk"] = infos[n]["qk"]
            m = n - LOOK
            if m >= 0:
                tile, first, last_ = seq[m]
                ctx = ctxs[(tile[0], tile[1])]
                h3 = rest(tile, ctx, infos[m], first, last_)
                if last_:
                    qt_epilogue(tile[0], tile[1], ctx, h3)
        dbg["maskTM"] = (maskTM, [128, NT * 32], BF16, [("c", "dve", sc.cnt["dve"])])

    if stage >= 4:
        P2DONE = [("c", "pe", sc.cnt["pe"]), ("c", "act", sc.cnt["act"]), ("c", "dve", sc.cnt["dve"]),
                  ("c", "pool", sc.cnt["pool"])]
        at_h = None
        col = 0
        for si, (a_, b_) in enumerate(SPLITS):
            w = (b_ - a_) // NCORES

            def ld_attn(e, si=si, w=w, col=col):
                pid = e.partition_id()
                src_ = agout_l[si].ap().rearrange("(rt p) c -> p rt c", p=128)[:, :, bass.ds(pid * w, w)]
                return e.dma_start(out=attnT[:, :, col:col + w], in_=src_)
            at_h = sc.dma("pool", ld_attn, "attn", deps=[ag_h[si]] + P2DONE)
            col += w

        for b in range(8):
            bank_last[b] = None
        g4 = P2DONE
        xs4b = A("xs4b", [128, D], F32, at=offs["tmp40"])
        xb4b = A("xb4b", [128, D], BF16, at=offs["actT0"])
        xs4l, xb4l = [xs4, xs4b], [xb4, xb4b]
        xs4_fr = [None, None]
        xb4_fr = [None, None]
        ev4 = []
        h = None
        for ts in range(8):
            sl4 = ts % 2
            sq, r2, xin = norm_tile(("o", ts, sl4), xo_d.ap()[ts * 128:(ts + 1) * 128, :], xs4l[sl4], xb4l[sl4],
                                    ssq2[:, ts:ts + 1], rstd2[:, ts:ts + 1],
                                    extra_deps=[xb4_fr[sl4]] + g4, load_deps=[xs4_fr[sl4]] + g4)
            h = sc.op("dve", lambda e, ts=ts, sl4=sl4: e.scalar_tensor_tensor(
                out=xb4l[sl4][:, :], in0=xs4l[sl4][:, :], scalar=rstd2[:, ts:ts + 1], in1=gB[:, :],
                op0=ALU.mult, op1=ALU.mult), deps=[r2, sq] + g4)
            xs4_fr[sl4] = h
            hs = transposes(xb4l[sl4], tp_banks[sl4], h,
                            lambda half, ts=ts: hT[:, half * 8:(half + 1) * 8, ts * 128:(ts + 1) * 128], g4)
            xb4_fr[sl4] = hs[1][0]
            ev4 += [hs[0][1], hs[1][1]]
        xs4_free = h
        xb4_free = xb4_fr[0]
        A4_PE = hs[1][0]
        HT_READY = ev4[-2:]
        GB1_FREE = xs4_free


        wr_free = [None, None, None]
        wr_i = [0]

        def wload(parts):
            s_ = wr_i[0] % 3
            wr_i[0] += 1
            wt = wring[s_]
            h = None
            for dst_fn, src in parts:
                h = sc.dma("pool", lambda e, dst_fn=dst_fn, src=src, wt=wt: e.dma_start(out=dst_fn(wt), in_=src),
                           f"wr{s_}", deps=[wr_free[s_]] + g4)
            return s_, wt, h

        def v3(wt, nk, nc_, off=0):
            return wt[:, off:off + nk * nc_].rearrange("p (k c) -> p k c", c=nc_)

        pbank_i = [0]

        def nb():
            b = pbank_i[0] % 8
            pbank_i[0] += 1
            return b

        items = []
        S4 = {"mt_last": None, "xacc": None, "xb4_free": xb4_free, "gbfree": GB1_FREE}
        tmp_free = [None] * 4
        wg_src = wg_d.ap().rearrange("(k p) c -> p k c", p=128)
        wpa_src = wpa_d.ap().rearrange("(k p) c -> p k c", p=128)
        wpb_src = wpb_d.ap().rearrange("(k p) c -> p k c", p=128)
        wo_src = wo_d.ap().rearrange("(k p) c -> p k c", p=128)
        wu_src = wu_d.ap().rearrange("(k p) c -> p k c", p=128)
        wd_src = wd_d.ap().rearrange("(f p) c -> p f c", p=128)

        def parts_b(dc):
            c0 = dc * 128
            return [(lambda wt: v3(wt, 16, 128, 0), wg_src[:, :, c0:c0 + 128]),
                    (lambda wt: v3(wt, 16, 128, 2048), wg_src[:, :, D + c0:D + c0 + 128]),
                    (lambda wt: v3(wt, 8, 128, 4096), wpa_src[:, :, c0:c0 + 128]),
                    (lambda wt: v3(wt, 8, 128, 5120), wpb_src[:, :, c0:c0 + 128])]

        def cons_b(dc):
            def f(wt, hw):
                lastpe = None
                for tt in range(2):
                    tk = slice(tt * 512, (tt + 1) * 512)
                    bks = [nb() for _ in range(4)]
                    hh = []
                    for m in range(2):
                        wv = v3(wt, 16, 128, m * 2048)
                        b = bks[m]
                        l_ = None
                        for k in range(KC):
                            l_ = sc.op("pe", lambda e, b=b, k=k, wv=wv, tk=tk: e.matmul(
                                banks[b][:, :], lhsT=wv[:, k, :], rhs=hT[:, k, tk],
                                start=(k == 0), stop=(k == KC - 1)),
                                deps=[hw, bank_last[b]] + HT_READY, signal=(k == KC - 1))
                        hh.append(l_)
                    for m in range(2):
                        wv = v3(wt, 8, 128, 4096 + m * 1024)
                        b = bks[2 + m]
                        l_ = None
                        for h_ in range(8):
                            l_ = sc.op("pe", lambda e, b=b, h_=h_, wv=wv, m=m, tk=tk: e.matmul(
                                banks[b][:, :], lhsT=wv[:, h_, :],
                                rhs=attnT[:, 2 * h_ + m, tk], start=(h_ == 0), stop=(h_ == 7)),
                                deps=[hw, at_h, bank_last[b]], signal=(h_ == 7))
                        hh.append(l_)
                        lastpe = l_
                    sg = []
                    for m in range(2):
                        s1 = sc.op("act", lambda e, m=m, b=bks[m]: e.activation(
                            out=tmp4[m][:, :], in_=banks[b][:, :], func=AF.Sigmoid),
                            deps=[hh[m], tmp_free[m]])
                        bank_last[bks[m]] = s1
                        sg.append(s1)
                    ml = []
                    for m in range(2):
                        m1 = sc.op("dve", lambda e, m=m, b=bks[2 + m]: e.tensor_tensor(
                            out=tmp4[2 + m][:, :], in0=banks[b][:, :], in1=tmp4[m][:, :], op=ALU.mult),
                            deps=[hh[2 + m], sg[m], tmp_free[2 + m]])
                        bank_last[bks[2 + m]] = m1
                        tmp_free[m] = m1
                        ml.append(m1)
                    S4["mt_last"] = sc.op("pool", lambda e, tk=tk: e.tensor_tensor(
                        out=mT[:, dc, tk], in0=tmp4[2][:, :], in1=tmp4[3][:, :], op=ALU.add), deps=ml)
                    tmp_free[2] = S4["mt_last"]
                    tmp_free[3] = S4["mt_last"]
                return lastpe
            return f

        if stage >= 5:
            for dc in range(KC):
                items.append((parts_b(dc), cons_b(dc)))

        def parts_o(ct):
            return [(lambda wt, q=q: v3(wt, 16, 512)[:, q * 4:(q + 1) * 4, :],
                     wo_src[:, q * 4:(q + 1) * 4, ct * 512:(ct + 1) * 512]) for q in range(4)]

        def cons_o(ct):
            def f(wt, hw):
                if ct == 0:
                    P4B = [("c", "pe", sc.cnt["pe"]), ("c", "pool", sc.cnt["pool"]), ("c", "dve", sc.cnt["dve"]),
                           ("c", "act", sc.cnt["act"])]
                    for ts in range(8):
                        S4["xr_h"] = sc.dma("sp", lambda e, ts=ts: e.dma_start(
                            out=xres[:, ts, :], in_=xo_d.ap()[ts * 128:(ts + 1) * 128, :]), "xres", deps=P4B)
                wv = v3(wt, 16, 512)
                lastpe = None
                for ts in range(8):
                    b = nb()
                    l_ = None
                    for k in range(KC):
                        l_ = sc.op("pe", lambda e, b=b, k=k, ts=ts: e.matmul(
                            banks[b][:, :], lhsT=mT[:, k, ts * 128:(ts + 1) * 128], rhs=wv[:, k, :],
                            start=(k == 0), stop=(k == KC - 1)),
                            deps=[hw, S4["mt_last"], bank_last[b]], signal=(k == KC - 1))
                    lastpe = l_
                    S4["xacc"] = sc.op("dve", lambda e, b=b, ts=ts: e.tensor_tensor(
                        out=xres[:, ts, ct * 512:(ct + 1) * 512], in0=banks[b][:, :],
                        in1=xres[:, ts, ct * 512:(ct + 1) * 512], op=ALU.add), deps=[l_, S4["xr_h"]])
                    bank_last[b] = S4["xacc"]
                    S4.setdefault("xacc_o", {})[ts] = S4["xacc"]
                    S4.setdefault("pe_o", {})[ts] = l_
                S4["wout_pe"] = lastpe
                if ct == 3 and stage >= 7:
                    phase_4d()
                return lastpe
            return f

        hmT = mT

        def phase_4d():
            gb2 = sc.dma("sp", lambda e: e.dma_start(out=gB[:, :], in_=bcast(g2_d, 0, D)), "gb2",
                         deps=[S4["gbfree"]])
            sc.op("pool", lambda e: e.memset(ssq2[:, :], 0.0), deps=[S4["gbfree"]])
            z2 = ("c", "pool", sc.cnt["pool"])
            ev5 = []
            h = None
            for ts in range(8):
                sq, r2, xin = norm_tile(("m", ts, 0), None, None, xb4, ssq2[:, ts:ts + 1], rstd2[:, ts:ts + 1],
                                        extra_deps=[S4["xb4_free"], S4["xacc_o"][ts], z2], in_sbuf=xres[:, ts, :])
                h = sc.op("dve", lambda e, ts=ts: e.scalar_tensor_tensor(out=xb4[:, :], in0=xres[:, ts, :],
                                                                         scalar=rstd2[:, ts:ts + 1], in1=gB[:, :],
                                                                         op0=ALU.mult, op1=ALU.mult),
                          deps=[r2, sq, gb2])
                hs = transposes(xb4, tp_banks[ts % 2], h,
                                lambda half, ts=ts: hmT[:, half * 8:(half + 1) * 8, ts * 128:(ts + 1) * 128],
                                [S4["pe_o"][ts]])
                S4["xb4_free"] = hs[1][0]
                ev5 += [hs[0][1], hs[1][1]]
            S4["hm_ready"] = ev5[-2:]
            S4["gbfree"] = h

        if stage >= 6:
            for ct in range(4):
                items.append((parts_o(ct), cons_o(ct)))

        act_free = [None, None]
        rl_free = [None, None]
        act_ready = {}

        def parts_u(fg):
            return [(lambda wt, q=q: v3(wt, 16, 512)[:, q * 4:(q + 1) * 4, :],
                     wu_src[:, q * 4:(q + 1) * 4, fg * 512:(fg + 1) * 512]) for q in range(4)]

        def parts_d(fg):
            return [(lambda wt, q=q: v3(wt, 4, 2048)[:, q:q + 1, :],
                     wd_src[:, fg * 4 + q:fg * 4 + q + 1, :]) for q in range(4)]

        def cons_u(fg):
            def f(wt, hw):
                wv = v3(wt, 16, 512)
                a_ = actT[fg % 2]
                lastpe = None
                sqh = None
                for fc in range(4):
                    for tt in range(2):
                        tk = slice(tt * 512, (tt + 1) * 512)
                        b = nb()
                        l_ = None
                        for k in range(KC):
                            l_ = sc.op("pe", lambda e, b=b, k=k, fc=fc, tk=tk: e.matmul(
                                banks[b][:, :], lhsT=wv[:, k, fc * 128:(fc + 1) * 128], rhs=hmT[:, k, tk],
                                start=(k == 0), stop=(k == KC - 1)),
                                deps=[hw, bank_last[b]] + S4["hm_ready"], signal=(k == KC - 1))
                        lastpe = l_
                        r = (fc * 2 + tt) % 2
                        rl = sc.op("act", lambda e, b=b, r=r: e.activation(out=tmp4[r][:, :], in_=banks[b][:, :],
                                                                           func=AF.Relu),
                                   deps=[l_, rl_free[r]])
                        bank_last[b] = rl
                        sqh = sc.op("pool", lambda e, r=r, fc=fc, tk=tk: e.tensor_tensor(
                            out=a_[:, fc, tk], in0=tmp4[r][:, :], in1=tmp4[r][:, :], op=ALU.mult),
                            deps=[rl, act_free[fg % 2]])
                        rl_free[r] = sqh
                act_ready[fg] = sqh
                return lastpe
            return f

        def cons_d(fg):
            def f(wt, hw):
                wv = v3(wt, 4, 2048)
                a_ = actT[fg % 2]
                lastpe = None
                for ts in range(8):
                    for ct in range(4):
                        b = nb()
                        l_ = None
                        for fc in range(4):
                            l_ = sc.op("pe", lambda e, b=b, fc=fc, ts=ts, ct=ct: e.matmul(
                                banks[b][:, :], lhsT=a_[:, fc, ts * 128:(ts + 1) * 128],
                                rhs=wv[:, fc, ct * 512:(ct + 1) * 512], start=(fc == 0), stop=(fc == 3)),
                                deps=[hw, act_ready[fg], bank_last[b]], signal=(fc == 3))
                        lastpe = l_
                        S4["xacc"] = sc.op("dve", lambda e, b=b, ts=ts, ct=ct: e.tensor_tensor(
                            out=xres[:, ts, ct * 512:(ct + 1) * 512], in0=banks[b][:, :],
                            in1=xres[:, ts, ct * 512:(ct + 1) * 512], op=ALU.add), deps=[l_])
                        bank_last[b] = S4["xacc"]
                        S4.setdefault("xacc_d", {})[ts] = S4["xacc"]
                act_free[fg % 2] = lastpe
                return lastpe
            return f

        if stage >= 7:
            NFG = 16 if stage >= 8 else 2
            order = [("u", 0)]
            for fg in range(1, NFG):
                order += [("u", fg), ("d", fg - 1)]
            order.append(("d", NFG - 1))
            for kind, fg in order:
                if kind == "u":
                    items.append((parts_u(fg), cons_u(fg)))
                else:
                    items.append((parts_d(fg), cons_d(fg)))

        loaded = {}
        for n in range(min(3, len(items))):
            loaded[n] = wload(items[n][0])
        for m_ in range(len(items)):
            s_, wt, hw = loaded[m_]
            lp = items[m_][1](wt, hw)
            wr_free[s_] = lp
            if m_ + 3 < len(items):
                loaded[m_ + 3] = wload(items[m_ + 3][0])

        if stage >= 7:
            gb3 = sc.dma("sp", lambda e: e.dma_start(out=gB[:, :], in_=bcast(g3_d, 0, D)), "gb3",
                         deps=[S4["gbfree"]])
            sc.op("pool", lambda e: e.memset(ssq2[:, :], 0.0), deps=[S4["gbfree"]])
            z3 = ("c", "pool", sc.cnt["pool"])
            ysl = [xs4] + [A(f"ys{i}", [128, D], F32, at=offs[f"wring{i}"]) for i in range(3)]
            ys_free = [None] * 4
            ys_wr = [None] + [wr_free[i] for i in range(3)]
            ysth = []
            for ts in range(8):
                yb = ts % 4
                sq, r2, xin = norm_tile(("f", ts, 0), None, None, xb4, ssq2[:, ts:ts + 1], rstd2[:, ts:ts + 1],
                                        extra_deps=[S4["xacc_d"][ts], z3, S4["xb4_free"]],
                                        in_sbuf=xres[:, ts, :])
                h = sc.op("dve", lambda e, ts=ts, yb=yb: e.scalar_tensor_tensor(
                    out=ysl[yb][:, :], in0=xres[:, ts, :], scalar=rstd2[:, ts:ts + 1], in1=gB[:, :],
                    op0=ALU.mult, op1=ALU.mult), deps=[r2, sq, gb3, ys_free[yb], ys_wr[yb]])
                yst = sc.dma("sp", lambda e, ts=ts, yb=yb: e.dma_start(out=y_d.ap()[ts * 128:(ts + 1) * 128, :],
                                                                       in_=ysl[yb][:, :]), f"yst{yb}", deps=[h])
                ys_free[yb] = yst
                ysth.append(yst)
            final_waits.extend(ysth[-4:])
        dbg["x"] = 1

    dbg_out = {}
    if stage < 9:
        allc = [("c", e_, sc.cnt[e_]) for e_ in ("pe", "act", "dve", "pool") if sc.cnt[e_] > 0]
        alld = [("d", k_, v_) for k_, v_ in sc.dcnt.items()]
        items = {"qTa": (qTa, [128, S], BF16), "kTa": (kTa, [128, S], BF16), "qTb": (qTb, [128, S], BF16),
                 "kTb": (kTb, [128, S], BF16), "Vab": (Vab, [128, NT * 256], BF16), "flog": (flog, [128, NT], F32),
                 "kmT": (kmT, [128, 32], F32), "rstd": (rstd, [128, NT], F32)}
        if stage >= 2:
            items["ctm"] = (ctm, [128, NT], F32)
            items["maskTM"] = (maskTM, [128, NT * 32], BF16)
        if stage >= 5:
            items = {"mT": (mT, [128, KC * TOK], BF16), "attnT": (attnT, [128, 16 * TOK], BF16)}
        if stage >= 6:
            items = {"xres": (xres, [128, 8 * D], F32)}
        if stage >= 7:
            items = {"xres": (xres, [128, 8 * D], F32)}
        if stage == 4:
            items = {"attnT": (attnT, [128, 16 * TOK], BF16)}
            alld = [("d", k_, v_) for k_, v_ in sc.dcnt.items()]
        for nm, (t_, shp, dt_) in items.items():
            dd = nc.dram_tensor("dbg_" + nm, shp, dt_, kind="ExternalOutput")
            dbg_out[nm] = (shp, dt_)
            flat = t_[:, :] if len(t_.shape) == 2 else t_[:, :, :].rearrange("p a b -> p (a b)")
            hdl = sc.dma("sp", lambda e, dd=dd, flat=flat: e.dma_start(out=dd.ap(), in_=flat), "dbg",
                         deps=allc + alld)
            final_waits.append(hdl)
        if stage >= 2 and stage < 5:
            agin_d = agin_l[0]
            dd = nc.dram_tensor("dbg_agin", [256, SPLITS[0][1]], BF16, kind="ExternalOutput")
            dbg_out["agin"] = ([256, SPLITS[0][1]], BF16)
            for r_ in range(2):
                hdl = sc.dma("sp", lambda e, dd=dd, r_=r_: e.dma_start(
                    out=dd.ap()[r_ * 128:(r_ + 1) * 128, :], in_=agin_d.ap()[r_ * 128:(r_ + 1) * 128, :]), "dbg",
                    deps=allc + alld + ob_handles)
                final_waits.append(hdl)
    if not final_waits:
        final_waits = [("d", k_, v_) for k_, v_ in sc.dcnt.items()]
    sc.wait("sp", final_waits)

    from contextlib import ExitStack
    with ExitStack() as es:
        csem = {e_: es.enter_context(nc.semaphore("s_" + e_)) for e_ in ("pe", "act", "dve", "pool")}
        dsem = {k_: es.enter_context(nc.semaphore("d_" + k_)) for k_ in sc.dcnt}
        block = es.enter_context(nc.Block())

        def run(engobj, en):
            waited = {}
            for fn, deps, signal, ds_, inc in sc.streams[en]:
                for d in deps:
                    key = (d[0], d[1])
                    if waited.get(key, 0) >= d[2]:
                        continue
                    sem = csem[d[1]] if d[0] == "c" else dsem[d[1]]
                    engobj.wait_ge(sem, d[2])
                    waited[key] = d[2]
                if fn is None:
                    continue
                ins = fn(engobj)
                if signal:
                    ins.then_inc(csem[en], 1)
                if ds_ is not None:
                    ins.then_inc(dsem[ds_], inc)

        @block.tensor
        def _(e):
            run(e, "pe")

        @block.scalar
        def _(e):
            run(e, "act")

        @block.vector
        def _(e):
            run(e, "dve")

        @block.gpsimd
        def _(e):
            run(e, "pool")

        @block.sync
        def _(e):
            run(e, "sp")
    return nc, dbg_out


def _consts(c):
    import ml_dtypes
    slope = float(2.0 ** (-(c + 1)))
    p = np.arange(128)
    identf = np.eye(128, dtype=np.float32)
    e = np.zeros((36, 32, 128), dtype=np.float32)
    for n in range(32):
        e[n, n, :] = 1.0
    e[32:36] = 1.0
    chunk = np.arange(512) // 128
    oh = (chunk[None, :] == np.arange(4)[:, None]).astype(np.float32)
    qend = (512.0 * np.arange(NQT)[:, None] + 128.0 * np.arange(4)[None, :] + 127.0)
    rowm = (-slope * qend / SCALE)[:, :, None] * oh[None, :, :]
    tri = np.where(p[:, None] <= p[None, :], 0.0, -BIG).astype(np.float32)
    kps = (slope * (128.0 * np.arange(NT)[None, :] + p[:, None])).astype(np.float32)
    qoff = np.broadcast_to((-slope * 512.0 * np.arange(NQT))[None, :], (128, NQT)).astype(np.float32)
    aql = (-slope * np.arange(512, dtype=np.float32))[None, :].astype(np.float32)
    return {
        "c_identb": identf.astype(ml_dtypes.bfloat16),
        "c_identf": identf,
        "c_triu": (p[:, None] <= p[None, :]).astype(np.float32),
        "c_su": (np.arange(64)[:, None] < np.arange(64)[None, :]).astype(np.float32),
        "c_tri": tri,
        "c_e": e.reshape(36, 32 * 128).astype(ml_dtypes.bfloat16),
        "c_rowm": rowm.reshape(NQT * 4, 512).astype(ml_dtypes.bfloat16),
        "c_oh": oh,
        "c_trib": tri.astype(ml_dtypes.bfloat16),
        "c_kps": kps,
        "c_qoff": np.ascontiguousarray(qoff),
        "c_aql": aql,
    }


_CACHE = {}


def make_in_maps(x, norm_mix_g, w_in, b_forget, w_proj_a, w_proj_b, w_out, norm_mlp_g, w_up, w_down,
                 norm_final_g):
    f = np.float32
    x2 = np.ascontiguousarray(np.asarray(x, dtype=f).reshape(S, D))
    w_in = np.asarray(w_in, dtype=f)
    w_gate = np.ascontiguousarray(w_in[:, 6152:6152 + 2 * D])
    shared = {
        "x": x2,
        "w_gate": w_gate,
        "w_proj_a": np.ascontiguousarray(np.asarray(w_proj_a, dtype=f)),
        "w_proj_b": np.ascontiguousarray(np.asarray(w_proj_b, dtype=f)),
        "w_out": np.ascontiguousarray(np.asarray(w_out, dtype=f)),
        "w_up": np.ascontiguousarray(np.asarray(w_up, dtype=f)),
        "w_down": np.ascontiguousarray(np.asarray(w_down, dtype=f)),
        "g_mix": np.asarray(norm_mix_g, dtype=f).reshape(1, D),
        "g_mlp": np.asarray(norm_mlp_g, dtype=f).reshape(1, D),
        "g_final": np.asarray(norm_final_g, dtype=f).reshape(1, D),
    }
    in_maps = []
    for c in range(NCORES):
        cols = []
        for base_ in (0, 1024, 3072, 4096, 2048, 5120):
            cols.append(w_in[:, base_ + c * 128: base_ + (c + 1) * 128])
        cols.append(w_in[:, 6144 + c: 6144 + c + 1])
        m = dict(shared)
        m["w_qkv"] = np.ascontiguousarray(np.concatenate(cols, axis=1))
        m["x_own"] = np.ascontiguousarray(x2[own_rows(c)])
        m["b_for"] = np.asarray(b_forget, dtype=f)[c].reshape(1, 1)
        m.update(_consts(c))
        in_maps.append(m)
    return in_maps


def kernel(x, norm_mix_g, w_in, b_forget, w_proj_a, w_proj_b, w_out, norm_mlp_g, w_up, w_down, norm_final_g):
    if "nc" not in _CACHE:
        _CACHE["nc"] = build_nc(9)[0]
    nc = _CACHE["nc"]
    in_maps = make_in_maps(x, norm_mix_g, w_in, b_forget, w_proj_a, w_proj_b, w_out, norm_mlp_g, w_up,
                           w_down, norm_final_g)
    res = run_bass_kernel_spmd(nc, in_maps, core_ids=list(range(NCORES)))
    ys = [np.asarray(res.results[c]["y"], dtype=np.float32) for c in range(NCORES)]
    out = np.empty((S, D), dtype=np.float32)
    for c in range(NCORES):
        out[own_rows(c)] = ys[c]
    return out.reshape(1, S, D)
```

```python
import os
import numpy as np
import concourse.bass as bass
import concourse.mybir as mybir
from concourse.bass_utils import run_bass_kernel_spmd

F32 = mybir.dt.float32
BF16 = mybir.dt.bfloat16
U8 = mybir.dt.uint8
AF = mybir.ActivationFunctionType
ALU = mybir.AluOpType
AX = mybir.AxisListType

NCORES = 8
S = 8192
D = 2048
DH = 128
KC = D // 128
TOK = S // NCORES
DFF = 4 * D
EPS = 1e-6
SCALE = DH ** -0.5
BIG = 30000.0
NT = S // 128
NQT = S // 512
WQ = 6 * 128 + 1
SPLITS = [(0, 5120), (5120, 7168), (7168, 7680), (7680, 8192)]


def own_rows(c):
    rows = []
    for a, b in SPLITS:
        w = (b - a) // NCORES
        rows.append(np.arange(a + c * w, a + (c + 1) * w))
    return np.concatenate(rows)

STAGE = int(os.environ.get("MK_STAGE", "9"))


class Sched:
    ENGS = ("pe", "act", "dve", "pool", "sp")

    def __init__(self):
        self.streams = {e: [] for e in self.ENGS}
        self.cnt = {e: 0 for e in self.ENGS}
        self.dcnt = {}

    def op(self, eng, fn, deps=(), signal=True):
        deps = [d for d in deps if d is not None]
        h = None
        if signal:
            self.cnt[eng] += 1
            h = ("c", eng, self.cnt[eng])
        self.streams[eng].append((fn, deps, signal, None, 0))
        return h

    def dma(self, eng, fn, sem, deps=(), inc=16):
        deps = [d for d in deps if d is not None]
        self.dcnt[sem] = self.dcnt.get(sem, 0) + inc
        h = ("d", sem, self.dcnt[sem])
        self.streams[eng].append((fn, deps, False, sem, inc))
        return h

    def wait(self, eng, deps):
        deps = [d for d in deps if d is not None]
        self.streams[eng].append((None, deps, False, None, 0))


def build_nc(stage=STAGE):
    nc = bass.Bass("TRN2", target_bir_lowering=False)
    sc = Sched()

    def din(name, shape, dt=F32):
        return nc.dram_tensor(name, list(shape), dt, kind="ExternalInput")

    x_d = din("x", [S, D])
    xo_d = din("x_own", [TOK, D])
    wqkv_d = din("w_qkv", [D, WQ])
    wg_d = din("w_gate", [D, 2 * D])
    wpa_d = din("w_proj_a", [1024, D])
    wpb_d = din("w_proj_b", [1024, D])
    wo_d = din("w_out", [D, D])
    wu_d = din("w_up", [D, DFF])
    wd_d = din("w_down", [DFF, D])
    g1_d = din("g_mix", [1, D])
    g2_d = din("g_mlp", [1, D])
    g3_d = din("g_final", [1, D])
    bf_d = din("b_for", [1, 1])
    cidb_d = din("c_identb", [128, 128], BF16)
    cidf_d = din("c_identf", [128, 128])
    ctriu_d = din("c_triu", [128, 128])
    csu_d = din("c_su", [64, 64])
    ctri_d = din("c_tri", [128, 128])
    ce_d = din("c_e", [36, 32 * 128], BF16)
    crowm_d = din("c_rowm", [NQT * 4, 512], BF16)
    coh_d = din("c_oh", [4, 512])
    ctrib_d = din("c_trib", [128, 128], BF16)
    ckps_d = din("c_kps", [128, NT])
    cqoff_d = din("c_qoff", [128, NQT])
    caql_d = din("c_aql", [1, 512])
    y_d = nc.dram_tensor("y", [TOK, D], F32, kind="ExternalOutput")
    agin_l = [nc.dram_tensor(f"ag_in{i}", [256, SPLITS[i][1] - SPLITS[i][0]], BF16, kind="Internal")
              for i in range(len(SPLITS))]
    agout_l = [nc.dram_tensor(f"ag_out{i}", [NCORES * 256, SPLITS[i][1] - SPLITS[i][0]], BF16, kind="Internal")
               for i in range(len(SPLITS))]
    cdram_d = nc.dram_tensor("c_scr", [64, 128], F32, kind="Internal")
    dbg = {}
    if stage < 9:
        dbg_d = {}

    def bcast(handle, off, n):
        return bass.AP(handle, off, [[0, 128], [1, n]])

    arena_g = nc.sbuf_tensor("arena", [128, 212000], U8)
    arena_g.__enter__()
    base = nc.lookup_mloc("arena").addr
    cur = [0]

    def salloc(name, shape, dt, at=None):
        esz = 4 if dt == F32 else 2
        n = 1
        for s_ in shape[1:]:
            n *= s_
        nbytes = (n * esz + 31) // 32 * 32
        if at is None:
            off = cur[0]
            cur[0] += nbytes
        else:
            off = at
        assert off + nbytes <= 212000, (name, off, nbytes)
        return nc.alloc_sbuf_tensor_at(name, list(shape), dt, offset=base + off), off, nbytes

    offs = {}

    def A(name, shape, dt, at=None):
        t_, off_, _ = salloc(name, shape, dt, at)
        offs[name] = off_
        return t_

    identb = A("identb", [128, 128], BF16)
    identf = A("identf", [128, 128], F32)
    onesb = A("onesb", [128, 128], BF16)
    onesf = A("onesf", [128, 128], F32)
    triu = A("triu", [128, 128], F32)
    su64 = A("su64", [64, 64], F32)
    tri = A("tri", [128, 128], F32)
    gB = A("gB", [128, D], F32)
    ssq = A("ssq", [128, NT], F32)
    rstd = A("rstd", [128, NT], F32)
    flog = A("flog", [128, NT], F32)
    kmT = A("kmT", [128, 32], F32)
    kps = A("kps", [128, NT], F32)
    qoff = A("qoff", [128, NQT], F32)
    bfor = A("bfor", [128, 1], F32)
    ctm = A("ctm", [128, NT], F32)
    negc = A("negc", [128, NT], F32)
    sm1 = A("sm1", [128, NT], F32)
    sm2 = A("sm2", [128, NT], F32)
    sm3 = A("sm3", [128, NT], F32)
    cT = A("cT", [64, 128], F32)
    bm64 = A("bm64", [64, 64], F32)
    tot64 = A("tot64", [64, 1], F32)
    gsb = [A(f"gsb{i}", [128, 32], F32) for i in range(2)]
    mx8 = [A(f"mx8{i}", [128, 8], F32) for i in range(2)]
    selt = [A(f"selt{i}", [128, 32], F32) for i in range(2)]
    biasqt = [A(f"biasqt{i}", [128, NT], F32) for i in range(2)]
    ssq2 = A("ssq2", [128, 8], F32)
    rstd2 = A("rstd2", [128, 8], F32)
    maskTM = A("maskTM", [128, NT, 32], BF16)
    P_CONST_END = cur[0]

    qTa = A("qTa", [128, S], BF16)
    kTa = A("kTa", [128, S], BF16)
    qTb = A("qTb", [128, S], BF16)
    kTb = A("kTb", [128, S], BF16)
    Vab = A("Vab", [128, NT, 256], BF16)
    P12_END = cur[0]
    wqkv = A("wqkv", [128, KC, WQ], BF16)
    xs = [A(f"xs{i}", [128, D], F32) for i in range(3)]
    xb = [A(f"xb{i}", [128, D], BF16) for i in range(2)]
    hnT = [A(f"hnT{i}", [128, KC, 512], BF16) for i in range(2)]
    qf32 = A("qf32", [128, 512], F32)
    P1_END = cur[0]
    cur[0] = P12_END
    Esel = A("Esel", [128, 32 * 128], BF16)
    aql = A("aql", [128, 512], F32)
    tbuf = [A(f"tbuf{i}", [128, 512], F32) for i in range(4)]
    pT = [A(f"pT{i}", [128, 512], BF16) for i in range(4)]
    cqB = [A(f"cqB{i}", [128, 512], F32) for i in range(2)]
    obuf = [A(f"obuf{i}", [128, 512], BF16) for i in range(4)]
    rden = [A(f"rden{i}", [128, 512], F32) for i in range(2)]
    maskTq = [A(f"maskTq{i}", [128, 512], BF16) for i in range(2)]
    pT += [A(f"pT{i}", [128, 512], BF16) for i in range(4, 8)]
    dsum = [A(f"dsum{i}", [128, 512], F32) for i in range(2)]
    ones32 = A("ones32", [128, 128], F32)
    trib = A("trib", [128, 128], BF16)
    oh4 = A("oh4", [128, 512], F32)
    crefs = A("crefs", [128, NQT], F32)
    Rf = [A(f"Rf{i}", [128, 512], BF16) for i in range(2)]
    ones4 = A("ones4", [128, 128], BF16)
    P2_END = cur[0]
    cur[0] = P_CONST_END
    regAB = cur[0]
    hT = A("hT", [128, KC, TOK], BF16)
    attnT = A("attnT", [128, 16, TOK], BF16)
    xres = A("xres", [128, 8, D], F32, at=regAB)
    mT = A("mT", [128, KC, TOK], BF16)
    wring = [A(f"wring{i}", [128, 8192], BF16) for i in range(3)]
    actT = [A(f"actT{i}", [128, 4, TOK], BF16) for i in range(2)]
    tmp4 = [A(f"tmp4{i}", [128, 512], F32) for i in range(4)]
    xs4 = A("xs4", [128, D], F32)
    xb4 = A("xb4", [128, D], BF16)
    P4_END = cur[0]
    assert max(P1_END, P2_END, P4_END) <= 212000, (P1_END, P2_END, P4_END)

    banks = []
    for b in range(8):
        g_ = nc.psum_tensor(f"bank{b}", [128, 512], F32)
        banks.append(g_.__enter__())

    def bank_bf(b):
        return banks[b].bitcast(BF16) if hasattr(banks[b], "bitcast") else None

    bank_last = [None] * 8

    cl = []
    PRELOAD = []
    cl.append(sc.dma("sp", lambda e: e.dma_start(out=identb[:, :], in_=cidb_d.ap()), "const"))
    cl.append(sc.dma("sp", lambda e: e.dma_start(out=identf[:, :], in_=cidf_d.ap()), "const"))
    cl.append(sc.dma("sp", lambda e: e.dma_start(out=triu[:, :], in_=ctriu_d.ap()), "const"))
    cl.append(sc.dma("sp", lambda e: e.dma_start(out=su64[:, :], in_=csu_d.ap()), "const"))
    cl.append(sc.dma("sp", lambda e: e.dma_start(out=tri[:, :], in_=ctri_d.ap()), "const"))
    cl.append(sc.dma("sp", lambda e: e.dma_start(out=kps[:, :], in_=ckps_d.ap()), "const"))
    cl.append(sc.dma("sp", lambda e: e.dma_start(out=qoff[:, :], in_=cqoff_d.ap()), "const"))
    cl.append(sc.dma("sp", lambda e: e.dma_start(out=bfor[:, :], in_=bcast(bf_d, 0, 1)), "const"))
    cl.append(sc.dma("sp", lambda e: e.dma_start(out=gB[:, :], in_=bcast(g1_d, 0, D)), "const"))
    CONST = cl[-1]
    sc.op("pool", lambda e: e.memset(onesb[:, :], 1.0))
    sc.op("pool", lambda e: e.memset(onesf[:, :], 1.0))
    sc.op("pool", lambda e: e.memset(ssq[:, :], 0.0))
    sc.op("pool", lambda e: e.memset(ssq2[:, :], 0.0))
    POOLINIT = sc.op("pool", lambda e: e.memset(kmT[:, :], 0.0))
    wq_src = wqkv_d.ap().rearrange("(k p) c -> p k c", p=128)
    wq_hs = []
    for s_ in range(5):
        c0_, c1_ = (s_ * 128, (s_ + 1) * 128) if s_ < 4 else (512, WQ)
        wq_hs.append(sc.dma("pool", lambda e, c0_=c0_, c1_=c1_: e.dma_start(
            out=wqkv[:, :, c0_:c1_], in_=wq_src[:, :, c0_:c1_]), f"wq{s_}"))

    st = {"sq": {}, "xb": {}, "tr": {}, "ev": {}}

    def norm_tile(key, src_ap, xs_t, xb_t, ssq_col, rstd_col, extra_deps=(), load_deps=(), in_sbuf=None):
        if in_sbuf is None:
            ld = sc.dma("sp", lambda e: e.dma_start(out=xs_t[:, :], in_=src_ap), "xs_" + key[0] + str(key[2]),
                        deps=list(load_deps))
            xin = xs_t[:, :]
        else:
            ld = None
            xin = in_sbuf
        sq = sc.op("act", lambda e: e.activation(out=xb_t[:, :], in_=xin, func=AF.Square,
                                                 accum_out=ssq_col),
                   deps=[ld, POOLINIT] + list(extra_deps))
        r0 = sc.op("dve", lambda e: e.tensor_scalar(out=rstd_col, in0=ssq_col, scalar1=1.0 / D,
                                                    scalar2=EPS, op0=ALU.mult, op1=ALU.add), deps=[sq])
        r1 = sc.op("act", lambda e: e.activation(out=rstd_col, in_=rstd_col, func=AF.Sqrt), deps=[r0])
        r2 = sc.op("dve", lambda e: e.reciprocal(out=rstd_col, in_=rstd_col), deps=[r1])
        return sq, r2, xin

    tp_banks = [(0, 1), (2, 3)]
    pj_banks = [4, 5, 6]
    pj_i = [0]
    GPB = 7
    hn_readers = [None, None]
    xs_free = [None, None, None]
    xb_free = [None, None]
    ev_last = {}
    gate_state = {}

    p1_ld = {}

    def p1_load(i):
        s3 = i % 3
        p1_ld[i] = sc.dma("sp", lambda e: e.dma_start(out=xs[s3][:, :], in_=x_d.ap()[i * 128:(i + 1) * 128, :]),
                          f"xs_a{s3}", deps=[xs_free[s3]])

    def p1_A(i):
        sl = i % 2
        s3 = i % 3
        if i + 2 < NT:
            p1_load(i + 2)
        sq, r2, xin = norm_tile(("a", i, sl), None, None, xb[sl],
                                ssq[:, i:i + 1], rstd[:, i:i + 1],
                                extra_deps=[xb_free[sl], p1_ld[i]], in_sbuf=xs[s3][:, :])
        h = sc.op("dve", lambda e: e.scalar_tensor_tensor(out=xb[sl][:, :], in0=xs[s3][:, :],
                                                          scalar=rstd[:, i:i + 1], in1=gB[:, :],
                                                          op0=ALU.mult, op1=ALU.mult),
                  deps=[r2, CONST, sq])
        xs_free[s3] = h
        st["xb"][i] = h

    def transposes(src_xb, bankpair, dep, dst_fn, evdeps):
        hs = []
        for half in range(2):
            b = bankpair[half]
            pb = banks[b].bitcast(BF16)
            last = None
            for kk in range(8):
                k = half * 8 + kk
                last = sc.op("pe", lambda e, pb=pb, kk=kk, k=k: e.transpose(
                    out=pb[:, kk * 128:(kk + 1) * 128], in_=src_xb[:, k * 128:(k + 1) * 128],
                    identity=identb[:, :]),
                    deps=[dep, CONST, bank_last[b]], signal=(kk == 7))
            src = pb[:, :].rearrange("p (k t) -> p k t", t=128)
            if half == 0:
                ev = sc.op("act", lambda e, src=src: e.activation(out=dst_fn(0), in_=src, func=AF.Copy),
                           deps=[last] + list(evdeps))
            else:
                ev = sc.op("dve", lambda e, src=src: e.tensor_copy(out=dst_fn(1), in_=src),
                           deps=[last] + list(evdeps))
            bank_last[b] = ev
            hs.append((last, ev))
        return hs

    def p1_B(i):
        sl = i % 2
        g, sub = i // 4, i % 4
        hs = transposes(xb[sl], tp_banks[sl], st["xb"][i],
                        lambda half: hnT[g % 2][:, half * 8:(half + 1) * 8, sub * 128:(sub + 1) * 128],
                        [hn_readers[g % 2]])
        xb_free[sl] = hs[1][0]
        ev_last[i] = [hs[0][1], hs[1][1]]

    def next_pj():
        b = pj_banks[pj_i[0] % 3]
        pj_i[0] += 1
        return b

    def p1_C(g, s):
        hsl = hnT[g % 2]
        evd = ev_last[4 * g + 3] + ev_last[4 * g + 2] + ev_last[4 * g + 1] + ev_last[4 * g]
        b = next_pj()
        last = None
        for k in range(KC):
            last = sc.op("pe", lambda e, b=b, k=k: e.matmul(
                banks[b][:, :], lhsT=wqkv[:, k, s * 128:(s + 1) * 128], rhs=hsl[:, k, :],
                start=(k == 0), stop=(k == KC - 1)),
                deps=evd + [wq_hs[s], bank_last[b]], signal=(k == KC - 1))
        cols = slice(g * 512, (g + 1) * 512)
        if s == 0:
            e1 = sc.op("act", lambda e, b=b: e.activation(out=qf32[:, :], in_=banks[b][:, :], func=AF.Copy),
                       deps=[last, gate_state.get("qf_free"), gate_state.get("qf_rd")])
            e2 = sc.op("dve", lambda e: e.tensor_copy(out=qTa[:, cols], in_=qf32[:, :]), deps=[e1])
            bank_last[b] = e1
            gate_state["qf"] = e1
            gate_state["qf_rd"] = e2
        elif s == 1:
            ee = None
            for h2 in range(2):
                ee = sc.op("act", lambda e, b=b, h2=h2: e.activation(
                    out=kTa[:, g * 512 + h2 * 256: g * 512 + (h2 + 1) * 256],
                    in_=banks[b][:, h2 * 256:(h2 + 1) * 256], func=AF.Copy,
                    accum_out=kmT[:, 2 * g + h2: 2 * g + h2 + 1]), deps=[last, POOLINIT])
            bank_last[b] = ee
            gate_state["km"] = ee
        elif s == 2:
            ee = sc.op("dve", lambda e, b=b: e.tensor_copy(out=qTb[:, cols], in_=banks[b][:, :]), deps=[last])
            bank_last[b] = ee
        else:
            ee = sc.op("act", lambda e, b=b: e.activation(out=kTb[:, cols], in_=banks[b][:, :], func=AF.Copy),
                       deps=[last])
            bank_last[b] = ee
        b = next_pj()
        i = 4 * g + s
        last = None
        for k in range(KC):
            last = sc.op("pe", lambda e, b=b, k=k: e.matmul(
                banks[b][:, 0:257], lhsT=hsl[:, k, s * 128:(s + 1) * 128], rhs=wqkv[:, k, 512:769],
                start=(k == 0), stop=(k == KC - 1)),
                deps=evd + [wq_hs[4], bank_last[b]], signal=(k == KC - 1))
        if s == 3:
            hn_readers[g % 2] = last
        e1 = sc.op("dve", lambda e, b=b: e.tensor_copy(out=Vab[:, i, :], in_=banks[b][:, 0:256]), deps=[last])
        e2 = sc.op("act", lambda e, b=b: e.activation(out=flog[:, i:i + 1], in_=banks[b][:, 256:257],
                                                      func=AF.Copy), deps=[last, e1])
        bank_last[b] = e2
        if s == 1:
            glast = None
            for u in range(4):
                ti = 4 * g + u
                blk = ti // 2
                gs = gsb[u % 2]
                m8 = mx8[u % 2]
                se = selt[u % 2]
                d0 = sc.op("pool", lambda e, gs=gs: e.memset(gs[:, :], -BIG), deps=[gate_state.get(("gsrd", u % 2))])
                if blk > 0:
                    gm = sc.op("pe", lambda e, u=u, blk=blk: e.matmul(
                        banks[GPB][:, u * 32:u * 32 + blk], lhsT=qf32[:, u * 128:(u + 1) * 128],
                        rhs=kmT[:, 0:blk], start=True, stop=True),
                        deps=[gate_state["qf"], gate_state["km"], bank_last[GPB]])
                    glast = gm
                    d0 = sc.op("dve", lambda e, gs=gs, u=u, blk=blk: e.tensor_copy(
                        out=gs[:, 0:blk], in_=banks[GPB][:, u * 32:u * 32 + blk]), deps=[gm, d0])
                    gate_state["gp_rd"] = d0
                    bank_last[GPB] = d0
                d1 = sc.op("dve", lambda e, gs=gs, m8=m8: e.max(out=m8[:, :], in_=gs[:, :]), deps=[d0])
                d2 = sc.op("dve", lambda e, m8=m8: e.tensor_scalar_max(out=m8[:, 2:3], in0=m8[:, 2:3],
                                                                        scalar1=-BIG / 2), deps=[d1])
                d3 = sc.op("dve", lambda e, gs=gs, m8=m8, se=se: e.tensor_scalar(
                    out=se[:, :], in0=gs[:, :], scalar1=m8[:, 2:3], scalar2=None, op0=ALU.is_ge), deps=[d2])
                d4 = sc.op("dve", lambda e, se=se, blk=blk: e.memset(se[:, blk:blk + 1], 1.0), deps=[d3])
                d5 = sc.op("dve", lambda e, se=se, ti=ti: e.tensor_scalar(
                    out=maskTM[:, ti, :], in0=se[:, :], scalar1=1.0, scalar2=BIG,
                    op0=ALU.subtract, op1=ALU.mult), deps=[d4])
                gate_state[("gsrd", u % 2)] = d5
            if glast is not None:
                gate_state["qf_free"] = glast

    NT1 = NT if stage >= 1 else 0
    if NT1:
        n_sp0 = len(sc.streams["sp"])
        p1_load(0)
        p1_load(1)
        first2 = sc.streams["sp"][n_sp0:n_sp0 + 2]
        del sc.streams["sp"][n_sp0:n_sp0 + 2]
        sc.streams["sp"][0:0] = first2
        p1_A(0)
        for i in range(NT1):
            if i + 1 < NT1:
                p1_A(i + 1)
            p1_B(i)
            if i >= 4:
                p1_C(i // 4 - 1, i % 4)
        for s in range(4):
            p1_C(NT1 // 4 - 1, s)

    final_waits = []
    ob_handles = []
    if stage >= 2:
        P1DONE_PE = ("c", "pe", sc.cnt["pe"])
        P1DONE_ACT = ("c", "act", sc.cnt["act"])
        P1DONE_DVE = ("c", "dve", sc.cnt["dve"])
        p1done = [P1DONE_PE, P1DONE_ACT, P1DONE_DVE]
        sc.op("pool", lambda e: e.memset(ones32[:, :], 1.0 / 32.0), deps=p1done)
        sc.op("pool", lambda e: e.memset(ones4[:, :], 0.0), deps=p1done)
        sc.op("pool", lambda e: e.memset(Rf[0][:, :], 0.0), deps=p1done)
        sc.op("pool", lambda e: e.memset(Rf[1][:, :], 0.0), deps=p1done)
        sc.op("pool", lambda e: e.memset(Esel[:, :], 0.0), deps=p1done)
        sc.op("pool", lambda e: e.memset(maskTq[0][:, :], 0.0), deps=p1done)
        sc.op("pool", lambda e: e.memset(maskTq[1][:, :], 0.0), deps=p1done)
        PAD0 = ("c", "pool", sc.cnt["pool"])
        c2 = sc.dma("sp", lambda e: e.dma_start(out=Esel[0:36, :], in_=ce_d.ap()), "const2", deps=p1done + [PAD0])
        c2 = sc.dma("sp", lambda e: e.dma_start(out=ones4[32:36, :], in_=ce_d.ap()[32:36, 0:128]), "const2",
                    deps=p1done + [PAD0])
        c2 = sc.dma("sp", lambda e: e.dma_start(out=trib[:, :], in_=ctrib_d.ap()), "const2", deps=p1done)
        c2 = sc.dma("sp", lambda e: e.dma_start(out=oh4[32:36, :], in_=coh_d.ap()), "const2", deps=p1done)
        c2 = sc.dma("sp", lambda e: e.dma_start(out=aql[:, :], in_=bcast(caql_d, 0, 512)), "const2", deps=p1done)
        CONST2 = c2
        for b in range(8):
            bank_last[b] = None

        z = sc.op("dve", lambda e: e.tensor_scalar(out=sm1[:, :], in0=flog[:, :], scalar1=bfor[:, 0:1],
                                                   scalar2=None, op0=ALU.add), deps=p1done + [CONST])
        az = sc.op("act", lambda e: e.activation(out=sm2[:, :], in_=sm1[:, :], func=AF.Abs), deps=[z] + p1done)
        ex = sc.op("act", lambda e: e.activation(out=sm2[:, :], in_=sm2[:, :], func=AF.Exp, scale=-1.0),
                   deps=[az] + p1done)
        ln = sc.op("act", lambda e: e.activation(out=sm2[:, :], in_=sm2[:, :], func=AF.Ln, bias=1.0),
                   deps=[ex])
        mn = sc.op("dve", lambda e: e.tensor_scalar_min(out=sm1[:, :], in0=sm1[:, :], scalar1=0.0), deps=[z, ln])
        lf = sc.op("dve", lambda e: e.tensor_sub(out=sm3[:, :], in0=sm1[:, :], in1=sm2[:, :]), deps=[mn, ln])
        t1 = sc.op("pe", lambda e: e.transpose(out=banks[0][0:64, 0:128], in_=sm3[:, :], identity=identf[:, :]),
                   deps=[lf] + p1done)
        tt_ = sc.op("dve", lambda e: e.reduce_sum(out=tot64[:, :], in_=banks[0][0:64, 0:128], axis=AX.X), deps=[t1])
        bmh = sc.op("dve", lambda e: e.tensor_scalar(out=bm64[:, :], in0=su64[:, :], scalar1=tot64[:, 0:1],
                                                     scalar2=None, op0=ALU.mult), deps=[tt_])
        sc.op("pe", lambda e: e.matmul(banks[1][:, 0:64], lhsT=triu[:, :], rhs=sm3[:, :], start=True, stop=False),
              deps=[lf], signal=False)
        cm = sc.op("pe", lambda e: e.matmul(banks[1][:, 0:64], lhsT=onesf[0:64, :], rhs=bm64[:, :],
                                            start=False, stop=True), deps=[bmh, POOLINIT])
        cc = sc.op("dve", lambda e: e.tensor_copy(out=ctm[:, :], in_=banks[1][:, 0:64]), deps=[cm])
        ng = sc.op("dve", lambda e: e.tensor_scalar_mul(out=negc[:, :], in0=ctm[:, :], scalar1=-1.0), deps=[cc])
        t2 = sc.op("pe", lambda e: e.transpose(out=banks[0][0:64, 0:128], in_=ctm[:, :], identity=identf[:, :]),
                   deps=[cc, tt_])
        ct_h = sc.op("dve", lambda e: e.tensor_copy(out=cT[:, :], in_=banks[0][0:64, 0:128]), deps=[t2])
        cst = sc.dma("sp", lambda e: e.dma_start(out=cdram_d.ap(), in_=cT[:, :]), "cst", deps=[ct_h])
        crf = sc.dma("sp", lambda e: e.dma_start(out=crefs[32:36, :],
                                                 in_=bass.AP(cdram_d, 127, [[128, 4], [512, NQT]]),
                                                 allow_slow_non_contiguous=True),
                     "crf", deps=[cst])
        bank_last[0] = ct_h
        bank_last[1] = ng
        dbg["ctm"] = (ctm, [128, NT], F32, [ng])

        SBK = [0, 1, 2, 3]
        OB = {0: 4, 1: 6}
        DB = {0: 5, 1: 7}
        sb_i = [0]
        t_rd = [None] * 4
        p_rd = [None] * 8
        pend = {0: [], 1: []}
        dsum_free = [None, None]
        cq_rd = [None, None]
        mq_rd = [None, None]
        bq_rd = [None, None]
        ob_free = [None] * 4
        rd_free = [None, None]
        ring_i = [0]

        sb_owner = {b_: None for b_ in SBK}

        def take_sb():
            for t_ in range(4):
                b_ = SBK[(sb_i[0] + t_) % 4]
                if sb_owner[b_] is None:
                    sb_i[0] = (sb_i[0] + t_ + 1) % 4
                    sb_owner[b_] = 1
                    return b_
            raise AssertionError("no free S^T bank")

        def tiles_for(typ, QT):
            return [(typ, QT, j) for j in range(4 * QT + 4)]

        def qt_prologue(typ, QT):
            sl = QT % 2
            if typ == 0:
                h = sc.op("pool", lambda e: e.tensor_scalar(out=Rf[sl][32:36, :], in0=oh4[32:36, :],
                                                            scalar1=crefs[32:36, QT:QT + 1], scalar2=1.0 / SCALE,
                                                            op0=ALU.mult, op1=ALU.mult),
                          deps=[crf, CONST2, cq_rd[sl]])
                return {"rf": Rf[sl], "rfh": h, "bias": negc, "biash": ng}
            b = take_sb()
            sb_owner[b] = None
            pb = banks[b].bitcast(BF16)
            last = None
            for u in range(4):
                last = sc.op("pe", lambda e, u=u, pb=pb: e.transpose(
                    out=pb[0:32, u * 128:(u + 1) * 128], in_=maskTM[:, 4 * QT + u, :], identity=identb[:, :]),
                    deps=[bank_last[b]] + p1done, signal=(u == 3))
            mh = sc.op("dve", lambda e, pb=pb: e.tensor_copy(out=maskTq[sl][0:32, :], in_=pb[0:32, 0:512]),
                       deps=[last, mq_rd[sl], PAD0])
            bank_last[b] = mh
            rh = sc.dma("sp", lambda e: e.dma_start(out=maskTq[sl][32:36, :], in_=crowm_d.ap()[4 * QT:4 * QT + 4, :]),
                        f"rowm{sl}", deps=[mq_rd[sl], PAD0])
            return {"bias": kps, "biash": CONST, "mq": maskTq[sl], "mqh": mh, "rmh": rh}

        def qk(tile, ctx):
            typ, QT, j = tile
            b = take_sb()
            jj = j - 4 * QT
            q0 = 128 * jj if jj > 0 else 0
            diag = jj >= 0
            kT_ = kTb if typ == 0 else kTa
            qT_ = qTb if typ == 0 else qTa
            sc.op("pe", lambda e: e.matmul(banks[b][:, q0:512], lhsT=kT_[:, j * 128:(j + 1) * 128],
                                           rhs=qT_[:, QT * 512 + q0:(QT + 1) * 512],
                                           start=True, stop=False),
                  deps=[bank_last[b]] + p1done, signal=False)
            if typ == 0:
                h = sc.op("pe", lambda e: e.matmul(banks[b][:, q0:512], lhsT=ones4[:, :],
                                                   rhs=ctx["rf"][:, q0:512], start=False, stop=not diag),
                          deps=[ctx["rfh"], CONST2, PAD0], signal=not diag)
            else:
                n = j // 2
                h = sc.op("pe", lambda e: e.matmul(banks[b][:, q0:512], lhsT=Esel[:, n * 128:(n + 1) * 128],
                                                   rhs=ctx["mq"][:, q0:512], start=False, stop=not diag),
                          deps=[ctx["mqh"], ctx["rmh"], CONST2, PAD0], signal=not diag)
            if diag:
                h = sc.op("pe", lambda e: e.matmul(banks[b][:, q0:q0 + 128], lhsT=identb[:, :], rhs=trib[:, :],
                                                   start=False, stop=True), deps=[CONST, CONST2])
            return {"b": b, "q0": q0, "jj": jj, "qk": h}

        def rest(tile, ctx, info, first, last_):
            typ, QT, j = tile
            b, q0, jj = info["b"], info["q0"], info["jj"]
            r = ring_i[0] % 4
            rp = ring_i[0] % 8
            ring_i[0] += 1
            pt = pT[rp]
            h2 = sc.op("act", lambda e: e.activation(out=pt[:, q0:512], in_=banks[b][:, q0:512], func=AF.Exp,
                                                     bias=ctx["bias"][:, j:j + 1], scale=SCALE),
                       deps=[info["qk"], ctx["biash"], p_rd[rp]])
            bank_last[b] = h2
            sb_owner[b] = None
            ctx["bias_rd"] = h2
            vsl = slice(128, 256) if typ == 0 else slice(0, 128)
            sc.op("pe", lambda e: e.matmul(banks[OB[typ]][:, q0:512], lhsT=Vab[:, j, vsl], rhs=pt[:, q0:512],
                                           start=first, stop=last_),
                  deps=[h2, bank_last[OB[typ]] if first else None], signal=False)
            if QT == 0:
                h3 = sc.op("pe", lambda e: e.matmul(banks[DB[typ]][:, q0:512], lhsT=onesb[:, :], rhs=pt[:, q0:512],
                                                    start=first, stop=last_),
                           deps=[h2, POOLINIT, bank_last[DB[typ]] if first else None])
                p_rd[rp] = h3
                return h3
            pend[typ].append((pt, q0, rp, h2, j))
            h3 = None
            if j % 4 == 3:
                for g_, (pt_, q0_, rp_, h2_, j_) in enumerate(pend[typ]):
                    h3 = sc.op("pe", lambda e, g_=g_, pt_=pt_, q0_=q0_, j_=j_: e.matmul(
                        banks[DB[typ]][32 * g_:32 * g_ + 32, q0_:512], lhsT=onesb[:, 32 * g_:32 * g_ + 32],
                        rhs=pt_[:, q0_:512], start=(j_ < 4), stop=(j_ >= 4 * QT),
                        tile_position=(0, 32 * g_), skip_group_check=True),
                        deps=[h2_, POOLINIT, bank_last[DB[typ]] if j_ < 4 else None], signal=(g_ == 3))
                for (_, _, rp_, _, _) in pend[typ]:
                    p_rd[rp_] = h3
                pend[typ] = []
            return h3

        oi = [0]
        seg_done = {}
        ag_h = []

        def qt_epilogue(typ, QT, ctx, lastpv):
            sl = typ
            o = oi[0] % 4
            oi[0] += 1
            if QT > 0:
                c1 = sc.op("dve", lambda e: e.tensor_copy(out=dsum[sl][:, :], in_=banks[DB[typ]][:, :]),
                           deps=[lastpv, dsum_free[sl]])
                lastpv = sc.op("pe", lambda e: e.matmul(banks[DB[typ]][:, :], lhsT=ones32[:, :], rhs=dsum[sl][:, :],
                                                        start=True, stop=True), deps=[c1, PAD0])
                dsum_free[sl] = lastpv
            r1 = sc.op("dve", lambda e: e.reciprocal(out=rden[sl][:, :], in_=banks[DB[typ]][:, :]),
                       deps=[lastpv, rd_free[sl]])
            r2 = sc.op("dve", lambda e: e.tensor_tensor(out=obuf[o][:, :], in0=banks[OB[typ]][:, :],
                                                        in1=rden[sl][:, :], op=ALU.mult),
                       deps=[r1, ob_free[o]])
            rd_free[sl] = r2
            bank_last[DB[typ]] = r1
            bank_last[OB[typ]] = r2
            row0 = 0 if typ == 1 else 128
            si = [i for i, (a_, b_) in enumerate(SPLITS) if a_ <= QT * 512 < b_][0]
            c0 = QT * 512 - SPLITS[si][0]
            dh = sc.dma("sp", lambda e: e.dma_start(out=agin_l[si].ap()[row0:row0 + 128, c0:c0 + 512],
                                                    in_=obuf[o][:, :]), f"ob{o}", deps=[r2])
            ob_free[o] = dh
            ob_handles.append(dh)
            seg_done[(typ, QT)] = True
            if (QT + 1) * 512 == SPLITS[si][1] and seg_done.get((1 - typ, QT)):
                ag_h.append(sc.dma("pool", lambda e: e.collective_compute(
                    "AllGather", ALU.bypass, replica_groups=[list(range(NCORES))],
                    ins=[agin_l[si].ap()], outs=[agout_l[si].ap()]), "cc",
                    deps=[h_ for h_ in ob_free if h_ is not None], inc=1))
            if typ == 0:
                cq_rd[QT % 2] = ctx["last_qk"]
            else:
                mq_rd[QT % 2] = ctx["last_qk"]
                bq_rd[QT % 2] = ctx["bias_rd"]

        seq = []
        nqt = NQT if stage >= 3 else 2
        for QT in range(nqt):
            for typ in (0, 1):
                tl = tiles_for(typ, QT)
                for idx, t in enumerate(tl):
                    seq.append((t, idx == 0, idx == len(tl) - 1))
        LOOK = 3
        HOIST = 6
        ctxs = {}
        infos = {}

        def maybe_prologue(k, force):
            if k >= len(seq):
                return
            tile, first, _ = seq[k]
            key = (tile[0], tile[1])
            if not first or key in ctxs:
                return
            if force or tile[1] == 0 or seg_done.get((tile[0], tile[1] - 1)):
                ctxs[key] = qt_prologue(tile[0], tile[1])

        for n in range(len(seq) + LOOK):
            if n < len(seq):
                for k_ in range(n + 1, n + HOIST + 1):
                    maybe_prologue(k_, False)
                maybe_prologue(n, True)
                tile, first, last_ = seq[n]
                ctx = ctxs[(tile[0], tile[1])]
                infos[n] = qk(tile, ctx)
                ctx["last_qk"] = infos[n]["qk"]
            m = n - LOOK
            if m >= 0:
                tile, first, last_ = seq[m]
                ctx = ctxs[(tile[0], tile[1])]
                h3 = rest(tile, ctx, infos[m], first, last_)
                if last_:
                    qt_epilogue(tile[0], tile[1], ctx, h3)
        dbg["maskTM"] = (maskTM, [128, NT * 32], BF16, [("c", "dve", sc.cnt["dve"])])

    if stage >= 4:
        P2DONE = [("c", "pe", sc.cnt["pe"]), ("c", "act", sc.cnt["act"]), ("c", "dve", sc.cnt["dve"]),
                  ("c", "pool", sc.cnt["pool"])]
        at_h = None
        col = 0
        for si, (a_, b_) in enumerate(SPLITS):
            w = (b_ - a_) // NCORES

            def ld_attn(e, si=si, w=w, col=col):
                pid = e.partition_id()
                src_ = agout_l[si].ap().rearrange("(rt p) c -> p rt c", p=128)[:, :, bass.ds(pid * w, w)]
                return e.dma_start(out=attnT[:, :, col:col + w], in_=src_)
            at_h = sc.dma("pool", ld_attn, "attn", deps=[ag_h[si]] + P2DONE)
            col += w

        for b in range(8):
            bank_last[b] = None
        g4 = P2DONE
        xs4b = A("xs4b", [128, D], F32, at=offs["tmp40"])
        xb4b = A("xb4b", [128, D], BF16, at=offs["actT0"])
        xs4l, xb4l = [xs4, xs4b], [xb4, xb4b]
        xs4_fr = [None, None]
        xb4_fr = [None, None]
        ev4 = []
        h = None
        for ts in range(8):
            sl4 = ts % 2
            sq, r2, xin = norm_tile(("o", ts, sl4), xo_d.ap()[ts * 128:(ts + 1) * 128, :], xs4l[sl4], xb4l[sl4],
                                    ssq2[:, ts:ts + 1], rstd2[:, ts:ts + 1],
                                    extra_deps=[xb4_fr[sl4]] + g4, load_deps=[xs4_fr[sl4]] + g4)
            h = sc.op("dve", lambda e, ts=ts, sl4=sl4: e.scalar_tensor_tensor(
                out=xb4l[sl4][:, :], in0=xs4l[sl4][:, :], scalar=rstd2[:, ts:ts + 1], in1=gB[:, :],
                op0=ALU.mult, op1=ALU.mult), deps=[r2, sq] + g4)
            xs4_fr[sl4] = h
            hs = transposes(xb4l[sl4], tp_banks[sl4], h,
                            lambda half, ts=ts: hT[:, half * 8:(half + 1) * 8, ts * 128:(ts + 1) * 128], g4)
            xb4_fr[sl4] = hs[1][0]
            ev4 += [hs[0][1], hs[1][1]]
        xs4_free = h
        xb4_free = xb4_fr[0]
        A4_PE = hs[1][0]
        HT_READY = ev4[-2:]
        GB1_FREE = xs4_free


        wr_free = [None, None, None]
        wr_i = [0]

        def wload(parts):
            s_ = wr_i[0] % 3
            wr_i[0] += 1
            wt = wring[s_]
            h = None
            for dst_fn, src in parts:
                h = sc.dma("pool", lambda e, dst_fn=dst_fn, src=src, wt=wt: e.dma_start(out=dst_fn(wt), in_=src),
                           f"wr{s_}", deps=[wr_free[s_]] + g4)
            return s_, wt, h

        def v3(wt, nk, nc_, off=0):
            return wt[:, off:off + nk * nc_].rearrange("p (k c) -> p k c", c=nc_)

        pbank_i = [0]

        def nb():
            b = pbank_i[0] % 8
            pbank_i[0] += 1
            return b

        items = []
        S4 = {"mt_last": None, "xacc": None, "xb4_free": xb4_free, "gbfree": GB1_FREE}
        tmp_free = [None] * 4
        wg_src = wg_d.ap().rearrange("(k p) c -> p k c", p=128)
        wpa_src = wpa_d.ap().rearrange("(k p) c -> p k c", p=128)
        wpb_src = wpb_d.ap().rearrange("(k p) c -> p k c", p=128)
        wo_src = wo_d.ap().rearrange("(k p) c -> p k c", p=128)
        wu_src = wu_d.ap().rearrange("(k p) c -> p k c", p=128)
        wd_src = wd_d.ap().rearrange("(f p) c -> p f c", p=128)

        def parts_b(dc):
            c0 = dc * 128
            return [(lambda wt: v3(wt, 16, 128, 0), wg_src[:, :, c0:c0 + 128]),
                    (lambda wt: v3(wt, 16, 128, 2048), wg_src[:, :, D + c0:D + c0 + 128]),
                    (lambda wt: v3(wt, 8, 128, 4096), wpa_src[:, :, c0:c0 + 128]),
                    (lambda wt: v3(wt, 8, 128, 5120), wpb_src[:, :, c0:c0 + 128])]

        def cons_b(dc):
            def f(wt, hw):
                lastpe = None
                for tt in range(2):
                    tk = slice(tt * 512, (tt + 1) * 512)
                    bks = [nb() for _ in range(4)]
                    hh = []
                    for m in range(2):
                        wv = v3(wt, 16, 128, m * 2048)
                        b = bks[m]
                        l_ = None
                        for k in range(KC):
                            l_ = sc.op("pe", lambda e, b=b, k=k, wv=wv, tk=tk: e.matmul(
                                banks[b][:, :], lhsT=wv[:, k, :], rhs=hT[:, k, tk],
                                start=(k == 0), stop=(k == KC - 1)),
                                deps=[hw, bank_last[b]] + HT_READY, signal=(k == KC - 1))
                        hh.append(l_)
                    for m in range(2):
                        wv = v3(wt, 8, 128, 4096 + m * 1024)
                        b = bks[2 + m]
                        l_ = None
                        for h_ in range(8):
                            l_ = sc.op("pe", lambda e, b=b, h_=h_, wv=wv, m=m, tk=tk: e.matmul(
                                banks[b][:, :], lhsT=wv[:, h_, :],
                                rhs=attnT[:, 2 * h_ + m, tk], start=(h_ == 0), stop=(h_ == 7)),
                                deps=[hw, at_h, bank_last[b]], signal=(h_ == 7))
                        hh.append(l_)
                        lastpe = l_
                    sg = []
                    for m in range(2):
                        s1 = sc.op("act", lambda e, m=m, b=bks[m]: e.activation(
                            out=tmp4[m][:, :], in_=banks[b][:, :], func=AF.Sigmoid),
                            deps=[hh[m], tmp_free[m]])
                        bank_last[bks[m]] = s1
                        sg.append(s1)
                    ml = []
                    for m in range(2):
                        m1 = sc.op("dve", lambda e, m=m, b=bks[2 + m]: e.tensor_tensor(
                            out=tmp4[2 + m][:, :], in0=banks[b][:, :], in1=tmp4[m][:, :], op=ALU.mult),
                            deps=[hh[2 + m], sg[m], tmp_free[2 + m]])
                        bank_last[bks[2 + m]] = m1
                        tmp_free[m] = m1
                        ml.append(m1)
                    S4["mt_last"] = sc.op("pool", lambda e, tk=tk: e.tensor_tensor(
                        out=mT[:, dc, tk], in0=tmp4[2][:, :], in1=tmp4[3][:, :], op=ALU.add), deps=ml)
                    tmp_free[2] = S4["mt_last"]
                    tmp_free[3] = S4["mt_last"]
                return lastpe
            return f

        if stage >= 5:
            for dc in range(KC):
                items.append((parts_b(dc), cons_b(dc)))

        def parts_o(ct):
            return [(lambda wt, q=q: v3(wt, 16, 512)[:, q * 4:(q + 1) * 4, :],
                     wo_src[:, q * 4:(q + 1) * 4, ct * 512:(ct + 1) * 512]) for q in range(4)]

        def cons_o(ct):
            def f(wt, hw):
                if ct == 0:
                    P4B = [("c", "pe", sc.cnt["pe"]), ("c", "pool", sc.cnt["pool"]), ("c", "dve", sc.cnt["dve"]),
                           ("c", "act", sc.cnt["act"])]
                    for ts in range(8):
                        S4["xr_h"] = sc.dma("sp", lambda e, ts=ts: e.dma_start(
                            out=xres[:, ts, :], in_=xo_d.ap()[ts * 128:(ts + 1) * 128, :]), "xres", deps=P4B)
                wv = v3(wt, 16, 512)
                lastpe = None
                for ts in range(8):
                    b = nb()
                    l_ = None
                    for k in range(KC):
                        l_ = sc.op("pe", lambda e, b=b, k=k, ts=ts: e.matmul(
                            banks[b][:, :], lhsT=mT[:, k, ts * 128:(ts + 1) * 128], rhs=wv[:, k, :],
                            start=(k == 0), stop=(k == KC - 1)),
                            deps=[hw, S4["mt_last"], bank_last[b]], signal=(k == KC - 1))
                    lastpe = l_
                    S4["xacc"] = sc.op("dve", lambda e, b=b, ts=ts: e.tensor_tensor(
                        out=xres[:, ts, ct * 512:(ct + 1) * 512], in0=banks[b][:, :],
                        in1=xres[:, ts, ct * 512:(ct + 1) * 512], op=ALU.add), deps=[l_, S4["xr_h"]])
                    bank_last[b] = S4["xacc"]
                    S4.setdefault("xacc_o", {})[ts] = S4["xacc"]
                    S4.setdefault("pe_o", {})[ts] = l_
                S4["wout_pe"] = lastpe
                if ct == 3 and stage >= 7:
                    phase_4d()
                return lastpe
            return f

        hmT = mT

        def phase_4d():
            gb2 = sc.dma("sp", lambda e: e.dma_start(out=gB[:, :], in_=bcast(g2_d, 0, D)), "gb2",
                         deps=[S4["gbfree"]])
            sc.op("pool", lambda e: e.memset(ssq2[:, :], 0.0), deps=[S4["gbfree"]])
            z2 = ("c", "pool", sc.cnt["pool"])
            ev5 = []
            h = None
            for ts in range(8):
                sq, r2, xin = norm_tile(("m", ts, 0), None, None, xb4, ssq2[:, ts:ts + 1], rstd2[:, ts:ts + 1],
                                        extra_deps=[S4["xb4_free"], S4["xacc_o"][ts], z2], in_sbuf=xres[:, ts, :])
                h = sc.op("dve", lambda e, ts=ts: e.scalar_tensor_tensor(out=xb4[:, :], in0=xres[:, ts, :],
                                                                         scalar=rstd2[:, ts:ts + 1], in1=gB[:, :],
                                                                         op0=ALU.mult, op1=ALU.mult),
                          deps=[r2, sq, gb2])
                hs = transposes(xb4, tp_banks[ts % 2], h,
                                lambda half, ts=ts: hmT[:, half * 8:(half + 1) * 8, ts * 128:(ts + 1) * 128],
                                [S4["pe_o"][ts]])
                S4["xb4_free"] = hs[1][0]
                ev5 += [hs[0][1], hs[1][1]]
            S4["hm_ready"] = ev5[-2:]
            S4["gbfree"] = h

        if stage >= 6:
            for ct in range(4):
                items.append((parts_o(ct), cons_o(ct)))

        act_free = [None, None]
        rl_free = [None, None]
        act_ready = {}

        def parts_u(fg):
            return [(lambda wt, q=q: v3(wt, 16, 512)[:, q * 4:(q + 1) * 4, :],
                     wu_src[:, q * 4:(q + 1) * 4, fg * 512:(fg + 1) * 512]) for q in range(4)]

        def parts_d(fg):
            return [(lambda wt, q=q: v3(wt, 4, 2048)[:, q:q + 1, :],
                     wd_src[:, fg * 4 + q:fg * 4 + q + 1, :]) for q in range(4)]

        def cons_u(fg):
            def f(wt, hw):
                wv = v3(wt, 16, 512)
                a_ = actT[fg % 2]
                lastpe = None
                sqh = None
                for fc in range(4):
                    for tt in range(2):
                        tk = slice(tt * 512, (tt + 1) * 512)
                        b = nb()
                        l_ = None
                        for k in range(KC):
                            l_ = sc.op("pe", lambda e, b=b, k=k, fc=fc, tk=tk: e.matmul(
                                banks[b][:, :], lhsT=wv[:, k, fc * 128:(fc + 1) * 128], rhs=hmT[:, k, tk],
                                start=(k == 0), stop=(k == KC - 1)),
                                deps=[hw, bank_last[b]] + S4["hm_ready"], signal=(k == KC - 1))
                        lastpe = l_
                        r = (fc * 2 + tt) % 2
                        rl = sc.op("act", lambda e, b=b, r=r: e.activation(out=tmp4[r][:, :], in_=banks[b][:, :],
                                                                           func=AF.Relu),
                                   deps=[l_, rl_free[r]])
                        bank_last[b] = rl
                        sqh = sc.op("pool", lambda e, r=r, fc=fc, tk=tk: e.tensor_tensor(
                            out=a_[:, fc, tk], in0=tmp4[r][:, :], in1=tmp4[r][:, :], op=ALU.mult),
                            deps=[rl, act_free[fg % 2]])
                        rl_free[r] = sqh
                act_ready[fg] = sqh
                return lastpe
            return f

        def cons_d(fg):
            def f(wt, hw):
                wv = v3(wt, 4, 2048)
                a_ = actT[fg % 2]
                lastpe = None
                for ts in range(8):
                    for ct in range(4):
                        b = nb()
                        l_ = None
                        for fc in range(4):
                            l_ = sc.op("pe", lambda e, b=b, fc=fc, ts=ts, ct=ct: e.matmul(
                                banks[b][:, :], lhsT=a_[:, fc, ts * 128:(ts + 1) * 128],
                                rhs=wv[:, fc, ct * 512:(ct + 1) * 512], start=(fc == 0), stop=(fc == 3)),
                                deps=[hw, act_ready[fg], bank_last[b]], signal=(fc == 3))
                        lastpe = l_
                        S4["xacc"] = sc.op("dve", lambda e, b=b, ts=ts, ct=ct: e.tensor_tensor(
                            out=xres[:, ts, ct * 512:(ct + 1) * 512], in0=banks[b][:, :],
                            in1=xres[:, ts, ct * 512:(ct + 1) * 512], op=ALU.add), deps=[l_])
                        bank_last[b] = S4["xacc"]
                        S4.setdefault("xacc_d", {})[ts] = S4["xacc"]
                act_free[fg % 2] = lastpe
                return lastpe
            return f

        if stage >= 7:
            NFG = 16 if stage >= 8 else 2
            order = [("u", 0)]
            for fg in range(1, NFG):
                order += [("u", fg), ("d", fg - 1)]
            order.append(("d", NFG - 1))
            for kind, fg in order:
                if kind == "u":
                    items.append((parts_u(fg), cons_u(fg)))
                else:
                    items.append((parts_d(fg), cons_d(fg)))

        loaded = {}
        for n in range(min(3, len(items))):
            loaded[n] = wload(items[n][0])
        for m_ in range(len(items)):
            s_, wt, hw = loaded[m_]
            lp = items[m_][1](wt, hw)
            wr_free[s_] = lp
            if m_ + 3 < len(items):
                loaded[m_ + 3] = wload(items[m_ + 3][0])

        if stage >= 7:
            gb3 = sc.dma("sp", lambda e: e.dma_start(out=gB[:, :], in_=bcast(g3_d, 0, D)), "gb3",
                         deps=[S4["gbfree"]])
            sc.op("pool", lambda e: e.memset(ssq2[:, :], 0.0), deps=[S4["gbfree"]])
            z3 = ("c", "pool", sc.cnt["pool"])
            ysl = [xs4] + [A(f"ys{i}", [128, D], F32, at=offs[f"wring{i}"]) for i in range(3)]
            ys_free = [None] * 4
            ys_wr = [None] + [wr_free[i] for i in range(3)]
            ysth = []
            for ts in range(8):
                yb = ts % 4
                sq, r2, xin = norm_tile(("f", ts, 0), None, None, xb4, ssq2[:, ts:ts + 1], rstd2[:, ts:ts + 1],
                                        extra_deps=[S4["xacc_d"][ts], z3, S4["xb4_free"]],
                                        in_sbuf=xres[:, ts, :])
                h = sc.op("dve", lambda e, ts=ts, yb=yb: e.scalar_tensor_tensor(
                    out=ysl[yb][:, :], in0=xres[:, ts, :], scalar=rstd2[:, ts:ts + 1], in1=gB[:, :],
                    op0=ALU.mult, op1=ALU.mult), deps=[r2, sq, gb3, ys_free[yb], ys_wr[yb]])
                yst = sc.dma("sp", lambda e, ts=ts, yb=yb: e.dma_start(out=y_d.ap()[ts * 128:(ts + 1) * 128, :],
                                                                       in_=ysl[yb][:, :]), f"yst{yb}", deps=[h])
                ys_free[yb] = yst
                ysth.append(yst)
            final_waits.extend(ysth[-4:])
        dbg["x"] = 1

    dbg_out = {}
    if stage < 9:
        allc = [("c", e_, sc.cnt[e_]) for e_ in ("pe", "act", "dve", "pool") if sc.cnt[e_] > 0]
        alld = [("d", k_, v_) for k_, v_ in sc.dcnt.items()]
        items = {"qTa": (qTa, [128, S], BF16), "kTa": (kTa, [128, S], BF16), "qTb": (qTb, [128, S], BF16),
                 "kTb": (kTb, [128, S], BF16), "Vab": (Vab, [128, NT * 256], BF16), "flog": (flog, [128, NT], F32),
                 "kmT": (kmT, [128, 32], F32), "rstd": (rstd, [128, NT], F32)}
        if stage >= 2:
            items["ctm"] = (ctm, [128, NT], F32)
            items["maskTM"] = (maskTM, [128, NT * 32], BF16)
        if stage >= 5:
            items = {"mT": (mT, [128, KC * TOK], BF16), "attnT": (attnT, [128, 16 * TOK], BF16)}
        if stage >= 6:
            items = {"xres": (xres, [128, 8 * D], F32)}
        if stage >= 7:
            items = {"xres": (xres, [128, 8 * D], F32)}
        if stage == 4:
            items = {"attnT": (attnT, [128, 16 * TOK], BF16)}
            alld = [("d", k_, v_) for k_, v_ in sc.dcnt.items()]
        for nm, (t_, shp, dt_) in items.items():
            dd = nc.dram_tensor("dbg_" + nm, shp, dt_, kind="ExternalOutput")
            dbg_out[nm] = (shp, dt_)
            flat = t_[:, :] if len(t_.shape) == 2 else t_[:, :, :].rearrange("p a b -> p (a b)")
            hdl = sc.dma("sp", lambda e, dd=dd, flat=flat: e.dma_start(out=dd.ap(), in_=flat), "dbg",
                         deps=allc + alld)
            final_waits.append(hdl)
        if stage >= 2 and stage < 5:
            agin_d = agin_l[0]
            dd = nc.dram_tensor("dbg_agin", [256, SPLITS[0][1]], BF16, kind="ExternalOutput")
            dbg_out["agin"] = ([256, SPLITS[0][1]], BF16)
            for r_ in range(2):
                hdl = sc.dma("sp", lambda e, dd=dd, r_=r_: e.dma_start(
                    out=dd.ap()[r_ * 128:(r_ + 1) * 128, :], in_=agin_d.ap()[r_ * 128:(r_ + 1) * 128, :]), "dbg",
                    deps=allc + alld + ob_handles)
                final_waits.append(hdl)
    if not final_waits:
        final_waits = [("d", k_, v_) for k_, v_ in sc.dcnt.items()]
    sc.wait("sp", final_waits)

    from contextlib import ExitStack
    with ExitStack() as es:
        csem = {e_: es.enter_context(nc.semaphore("s_" + e_)) for e_ in ("pe", "act", "dve", "pool")}
        dsem = {k_: es.enter_context(nc.semaphore("d_" + k_)) for k_ in sc.dcnt}
        block = es.enter_context(nc.Block())

        def run(engobj, en):
            waited = {}
            for fn, deps, signal, ds_, inc in sc.streams[en]:
                for d in deps:
                    key = (d[0], d[1])
                    if waited.get(key, 0) >= d[2]:
                        continue
                    sem = csem[d[1]] if d[0] == "c" else dsem[d[1]]
                    engobj.wait_ge(sem, d[2])
                    waited[key] = d[2]
                if fn is None:
                    continue
                ins = fn(engobj)
                if signal:
                    ins.then_inc(csem[en], 1)
                if ds_ is not None:
                    ins.then_inc(dsem[ds_], inc)

        @block.tensor
        def _(e):
            run(e, "pe")

        @block.scalar
        def _(e):
            run(e, "act")

        @block.vector
        def _(e):
            run(e, "dve")

        @block.gpsimd
        def _(e):
            run(e, "pool")

        @block.sync
        def _(e):
            run(e, "sp")
    return nc, dbg_out


def _consts(c):
    import ml_dtypes
    slope = float(2.0 ** (-(c + 1)))
    p = np.arange(128)
    identf = np.eye(128, dtype=np.float32)
    e = np.zeros((36, 32, 128), dtype=np.float32)
    for n in range(32):
        e[n, n, :] = 1.0
    e[32:36] = 1.0
    chunk = np.arange(512) // 128
    oh = (chunk[None, :] == np.arange(4)[:, None]).astype(np.float32)
    qend = (512.0 * np.arange(NQT)[:, None] + 128.0 * np.arange(4)[None, :] + 127.0)
    rowm = (-slope * qend / SCALE)[:, :, None] * oh[None, :, :]
    tri = np.where(p[:, None] <= p[None, :], 0.0, -BIG).astype(np.float32)
    kps = (slope * (128.0 * np.arange(NT)[None, :] + p[:, None])).astype(np.float32)
    qoff = np.broadcast_to((-slope * 512.0 * np.arange(NQT))[None, :], (128, NQT)).astype(np.float32)
    aql = (-slope * np.arange(512, dtype=np.float32))[None, :].astype(np.float32)
    return {
        "c_identb": identf.astype(ml_dtypes.bfloat16),
        "c_identf": identf,
        "c_triu": (p[:, None] <= p[None, :]).astype(np.float32),
        "c_su": (np.arange(64)[:, None] < np.arange(64)[None, :]).astype(np.float32),
        "c_tri": tri,
        "c_e": e.reshape(36, 32 * 128).astype(ml_dtypes.bfloat16),
        "c_rowm": rowm.reshape(NQT * 4, 512).astype(ml_dtypes.bfloat16),
        "c_oh": oh,
        "c_trib": tri.astype(ml_dtypes.bfloat16),
        "c_kps": kps,
        "c_qoff": np.ascontiguousarray(qoff),
        "c_aql": aql,
    }


_CACHE = {}


def make_in_maps(x, norm_mix_g, w_in, b_forget, w_proj_a, w_proj_b, w_out, norm_mlp_g, w_up, w_down,
                 norm_final_g):
    f = np.float32
    x2 = np.ascontiguousarray(np.asarray(x, dtype=f).reshape(S, D))
    w_in = np.asarray(w_in, dtype=f)
    w_gate = np.ascontiguousarray(w_in[:, 6152:6152 + 2 * D])
    shared = {
        "x": x2,
        "w_gate": w_gate,
        "w_proj_a": np.ascontiguousarray(np.asarray(w_proj_a, dtype=f)),
        "w_proj_b": np.ascontiguousarray(np.asarray(w_proj_b, dtype=f)),
        "w_out": np.ascontiguousarray(np.asarray(w_out, dtype=f)),
        "w_up": np.ascontiguousarray(np.asarray(w_up, dtype=f)),
        "w_down": np.ascontiguousarray(np.asarray(w_down, dtype=f)),
        "g_mix": np.asarray(norm_mix_g, dtype=f).reshape(1, D),
        "g_mlp": np.asarray(norm_mlp_g, dtype=f).reshape(1, D),
        "g_final": np.asarray(norm_final_g, dtype=f).reshape(1, D),
    }
    in_maps = []
    for c in range(NCORES):
        cols = []
        for base_ in (0, 1024, 3072, 4096, 2048, 5120):
            cols.append(w_in[:, base_ + c * 128: base_ + (c + 1) * 128])
        cols.append(w_in[:, 6144 + c: 6144 + c + 1])
        m = dict(shared)
        m["w_qkv"] = np.ascontiguousarray(np.concatenate(cols, axis=1))
        m["x_own"] = np.ascontiguousarray(x2[own_rows(c)])
        m["b_for"] = np.asarray(b_forget, dtype=f)[c].reshape(1, 1)
        m.update(_consts(c))
        in_maps.append(m)
    return in_maps


def kernel(x, norm_mix_g, w_in, b_forget, w_proj_a, w_proj_b, w_out, norm_mlp_g, w_up, w_down, norm_final_g):
    if "nc" not in _CACHE:
        _CACHE["nc"] = build_nc(9)[0]
    nc = _CACHE["nc"]
    in_maps = make_in_maps(x, norm_mix_g, w_in, b_forget, w_proj_a, w_proj_b, w_out, norm_mlp_g, w_up,
                           w_down, norm_final_g)
    res = run_bass_kernel_spmd(nc, in_maps, core_ids=list(range(NCORES)))
    ys = [np.asarray(res.results[c]["y"], dtype=np.float32) for c in range(NCORES)]
    out = np.empty((S, D), dtype=np.float32)
    for c in range(NCORES):
        out[own_rows(c)] = ys[c]
    return out.reshape(1, S, D)
```
